# Optimizing a Trainium2 kernel written in Bass

```python
import jax, jax.numpy as jnp
from jax import lax
import numpy as np

D_MODEL = 1024
BATCH = 8
SEQ = 2048
DEPTH = 4

MIX_WIDTH = D_MODEL
POOL_WIDTH = MIX_WIDTH // 2
POOL_WINDOWS = (2, 4, 8, 16)
N_POOL_GROUPS = len(POOL_WINDOWS)
POOL_GROUP = POOL_WIDTH // N_POOL_GROUPS
ATTN_WIDTH = MIX_WIDTH - POOL_WIDTH
HEAD_DIM = 64
N_HEADS = ATTN_WIDTH // HEAD_DIM
N_KV_HEADS = 2
GQA_GROUP = N_HEADS // N_KV_HEADS
WINDOW = 128
BLOCK = 128
Q_OFF = POOL_WIDTH
K_OFF = Q_OFF + N_HEADS * HEAD_DIM
V_OFF = K_OFF + N_KV_HEADS * HEAD_DIM
IN_WIDTH = V_OFF + N_KV_HEADS * HEAD_DIM

N_KEYS = 128
N_EXPERTS = N_KEYS * N_KEYS
PEER_HEADS = 8
PEER_TOPK = 16
D_KEY = 256
D_HALF = D_KEY // 2
PEER_CHUNK = 128

RMS_EPS = 1e-6
N_MOD = 6

kernel_name = "hybrid_pool_swa_peer_adaln_trunk"


def rms_norm(x, g):
    xf = x.astype(jnp.float32)
    y = xf * lax.rsqrt(jnp.mean(xf * xf, axis=-1, keepdims=True) + RMS_EPS)
    return (y * g.astype(jnp.float32)).astype(x.dtype)


def alibi_slopes(n_heads):
    return jnp.exp2(-8.0 * jnp.arange(1, n_heads + 1, dtype=jnp.float32) / n_heads)


def multiscale_pool(p, pool_w, pool_scale):
    B, S, _ = p.shape
    pg = p.reshape(B, S, N_POOL_GROUPS, POOL_GROUP)
    cs = jnp.cumsum(pg.astype(jnp.float32), axis=1)
    pos1 = jnp.arange(1, S + 1, dtype=jnp.float32)
    means = []
    for g, w in enumerate(POOL_WINDOWS):
        cg = cs[:, :, g]
        lag = jnp.pad(cg, ((0, 0), (w, 0), (0, 0)))[:, :S]
        means.append((cg - lag) / jnp.minimum(pos1, float(w))[None, :, None])
    pooled = jnp.stack(means, axis=2).astype(p.dtype) - pg
    mixed = jnp.einsum('bsgc,gcd->bsgd', pooled, pool_w)
    return mixed.reshape(B, S, POOL_WIDTH) * pool_scale


def band_blocks(t, nb):
    B = t.shape[0]
    tb = t.reshape(B, nb, BLOCK, N_KV_HEADS, HEAD_DIM)
    prev = jnp.pad(tb, ((0, 0), (1, 0), (0, 0), (0, 0), (0, 0)))[:, :nb]
    return jnp.concatenate([prev, tb], axis=2)


def sliding_window_attention(q, k, v, sinks):
    B, S = q.shape[:2]
    nb = S // BLOCK
    qb = q.reshape(B, nb, BLOCK, N_KV_HEADS, GQA_GROUP, HEAD_DIM)
    kb = band_blocks(k, nb)
    vb = band_blocks(v, nb)
    scores = jnp.einsum('bnqkgd,bnskd->bnkgqs', qb, kb,
                        preferred_element_type=jnp.float32) * (HEAD_DIM ** -0.5)
    qi = jnp.arange(BLOCK)[:, None]
    sj = jnp.arange(2 * BLOCK)[None, :]
    dist = qi + BLOCK - sj
    band_ok = (dist >= 0) & (dist < WINDOW)
    blk = jnp.arange(nb)[:, None, None]
    valid = band_ok[None] & ((blk > 0) | (sj[None] >= BLOCK))
    slopes = alibi_slopes(N_HEADS).reshape(N_KV_HEADS, GQA_GROUP)
    bias = -slopes[:, :, None, None] * dist.astype(jnp.float32)
    scores = jnp.where(valid[None, :, None, None], scores + bias[None, None], -jnp.inf)
    sink = sinks.astype(jnp.float32).reshape(N_KV_HEADS, GQA_GROUP)[None, None, :, :, None, None]
    sink = jnp.broadcast_to(sink, scores.shape[:-1] + (1,))
    probs = jax.nn.softmax(jnp.concatenate([scores, sink], axis=-1), axis=-1)[..., :-1]
    out = jnp.einsum('bnkgqs,bnskd->bnqkgd', probs.astype(v.dtype), vb)
    return out.reshape(B, S, N_HEADS * HEAD_DIM)


def peer_ffn(h, wq, subkeys, u, v):
    B, S, D = h.shape
    T = B * S
    hf = h.reshape(T, D)
    q = (hf @ wq).reshape(T, PEER_HEADS, 2, D_HALF)
    s = jnp.einsum('thpd,pnd->thpn', q, subkeys, preferred_element_type=jnp.float32)
    top_s, top_i = lax.top_k(s, PEER_TOPK)
    cand = top_s[:, :, 0, :, None] + top_s[:, :, 1, None, :]
    best_s, best_j = lax.top_k(cand.reshape(T, PEER_HEADS, PEER_TOPK * PEER_TOPK), PEER_TOPK)
    i1 = jnp.take_along_axis(top_i[:, :, 0], best_j // PEER_TOPK, axis=-1)
    i2 = jnp.take_along_axis(top_i[:, :, 1], best_j % PEER_TOPK, axis=-1)
    expert = (i1 * N_KEYS + i2).reshape(T, PEER_HEADS * PEER_TOPK)
    gate = jax.nn.softmax(best_s, axis=-1).reshape(T, PEER_HEADS * PEER_TOPK).astype(h.dtype)
    nc = T // PEER_CHUNK

    def expert_block(args):
        hc, ec, gc = args
        uc = jnp.take(u, ec, axis=0)
        a = jnp.einsum('cd,ced->ce', hc, uc)
        act = jax.nn.gelu(a, approximate=False) * gc
        vc = jnp.take(v, ec, axis=0)
        return jnp.einsum('ce,ced->cd', act, vc)

    out = lax.map(expert_block, (hf.reshape(nc, PEER_CHUNK, D),
                                 expert.reshape(nc, PEER_CHUNK, -1),
                                 gate.reshape(nc, PEER_CHUNK, -1)))
    return out.reshape(B, S, D)


def setup_inputs(seed: int = 0) -> dict:
    key = jax.random.key(seed)
    ks = jax.random.split(key, 20)
    f32 = jnp.float32
    nrm = lambda k, shape, s: jax.random.normal(k, shape, f32) * s
    return {
        "x": nrm(ks[0], (BATCH, SEQ, D_MODEL), 1.0),
        "c": nrm(ks[1], (BATCH, D_MODEL), 1.0),
        "w_ada": nrm(ks[2], (DEPTH, D_MODEL, N_MOD * D_MODEL), 0.5 * D_MODEL ** -0.5),
        "b_ada": nrm(ks[3], (DEPTH, N_MOD * D_MODEL), 0.02),
        "norm1_g": 1.0 + nrm(ks[4], (DEPTH, D_MODEL), 0.05),
        "norm2_g": 1.0 + nrm(ks[5], (DEPTH, D_MODEL), 0.05),
        "w_in": nrm(ks[6], (DEPTH, D_MODEL, IN_WIDTH), D_MODEL ** -0.5),
        "pool_w": nrm(ks[7], (DEPTH, N_POOL_GROUPS, POOL_GROUP, POOL_GROUP), POOL_GROUP ** -0.5),
        "pool_scale": 1.0 + nrm(ks[8], (DEPTH, POOL_WIDTH), 0.1),
        "q_norm_g": 1.0 + nrm(ks[9], (DEPTH, HEAD_DIM), 0.05),
        "k_norm_g": 1.0 + nrm(ks[10], (DEPTH, HEAD_DIM), 0.05),
        "attn_sinks": nrm(ks[11], (DEPTH, N_HEADS), 0.5),
        "mix_norm_g": 1.0 + nrm(ks[12], (DEPTH, MIX_WIDTH), 0.05),
        "w_out": nrm(ks[13], (DEPTH, MIX_WIDTH, D_MODEL), MIX_WIDTH ** -0.5),
        "peer_wq": nrm(ks[14], (DEPTH, D_MODEL, PEER_HEADS * D_KEY), D_MODEL ** -0.5),
        "peer_subkeys": nrm(ks[15], (DEPTH, 2, N_KEYS, D_HALF), D_HALF ** -0.5),
        "peer_u": nrm(ks[16], (DEPTH, N_EXPERTS, D_MODEL), D_MODEL ** -0.5),
        "peer_v": nrm(ks[17], (DEPTH, N_EXPERTS, D_MODEL), PEER_HEADS ** -0.5),
    }


def reference(x, c, w_ada, b_ada, norm1_g, norm2_g, w_in, pool_w, pool_scale, q_norm_g,
              k_norm_g, attn_sinks, mix_norm_g, w_out, peer_wq, peer_subkeys, peer_u, peer_v):
    B, S, D = x.shape
    cond = jax.nn.silu(c)
    for l in range(DEPTH):
        mod = (cond @ w_ada[l] + b_ada[l])[:, None, :]
        sh1, sc1, g1, sh2, sc2, g2 = jnp.split(mod, N_MOD, axis=-1)

        h = rms_norm(x, norm1_g[l]) * (1.0 + sc1) + sh1
        proj = h @ w_in[l]
        p = proj[..., :Q_OFF]
        q = proj[..., Q_OFF:K_OFF].reshape(B, S, N_HEADS, HEAD_DIM)
        k = proj[..., K_OFF:V_OFF].reshape(B, S, N_KV_HEADS, HEAD_DIM)
        v = proj[..., V_OFF:].reshape(B, S, N_KV_HEADS, HEAD_DIM)
        q = rms_norm(q, q_norm_g[l])
        k = rms_norm(k, k_norm_g[l])
        pool_out = multiscale_pool(p, pool_w[l], pool_scale[l])
        attn_out = sliding_window_attention(q, k, v, attn_sinks[l])
        y = jnp.concatenate([pool_out, attn_out], axis=-1).reshape(B, S, 2, MIX_WIDTH // 2)
        y = rms_norm(y, jnp.ones((MIX_WIDTH // 2,), jnp.float32)).reshape(B, S, MIX_WIDTH) * mix_norm_g[l]
        x = x + g1 * (y @ w_out[l])

        h2 = rms_norm(x, norm2_g[l]) * (1.0 + sc2) + sh2
        x = x + g2 * peer_ffn(h2, peer_wq[l], peer_subkeys[l], peer_u[l], peer_v[l])
    return x
```

```python
import numpy as np
from contextlib import ExitStack
import concourse.bass as bass
import concourse.mybir as mybir
from concourse.bass_utils import run_bass_kernel_spmd

F32 = mybir.dt.float32
BF16 = mybir.dt.bfloat16
AF = mybir.ActivationFunctionType
ALU = mybir.AluOpType
AX = mybir.AxisListType

NL = 4
S = 2048
D = 1024
EPS = 1e-6
NEG = -30000.0
THR_EPS = 4e-6


class Buf:
    __slots__ = ("name", "w", "r")

    def __init__(self, name=""):
        self.name = name
        self.w = None
        self.r = []


class Op:
    __slots__ = ("eng", "fn", "deps", "needs_inc", "ev", "is_dma", "tag", "idx")


class Prog:
    ENGS = ("pe", "act", "dve", "pool", "sp")

    def __init__(self, nc, stack):
        self.nc = nc
        self.stack = stack
        self.ops = []
        self.eng_obj = {"pe": nc.tensor, "act": nc.scalar, "dve": nc.vector,
                        "pool": nc.gpsimd, "sp": nc.sync}
        self.nsem = 0
        self.bufs = []
        self.last = {}
        self.dmas = []
        self.emitted = 0
        self.eng_sem = {}
        self.eng_cnt = {}
        self.tag_sem = {}
        self.tag_cnt = {}
        self.waited = {e: {} for e in self.ENGS}
        self.n_ops = 0

    def buf(self, name=""):
        b = Buf(name)
        self.bufs.append(b)
        return b

    def drop(self, bufs):
        s = set(id(b) for b in bufs)
        self.bufs = [b for b in self.bufs if id(b) not in s]

    def op(self, eng, fn, reads=(), writes=(), dma_tag=None, extra_deps=()):
        o = Op()
        o.eng = eng
        o.fn = fn
        o.needs_inc = False
        o.ev = None
        o.is_dma = dma_tag is not None
        o.tag = dma_tag
        o.idx = self.n_ops
        self.n_ops += 1
        deps = list(extra_deps)
        for b in reads:
            if b.w is not None:
                deps.append(b.w)
        for b in writes:
            if b.w is not None:
                deps.append(b.w)
            deps.extend(b.r)
        seen = set()
        dd = []
        for d in deps:
            if eng == "pe" and d.eng == "pe" and not d.is_dma:
                continue
            if d.idx not in seen and d is not o:
                seen.add(d.idx)
                dd.append(d)
        o.deps = dd
        for d in dd:
            d.needs_inc = True
        for b in reads:
            if not o.is_dma:
                b.r = [r for r in b.r if r.is_dma or r.eng != eng]
            b.r.append(o)
        for b in writes:
            b.w = o
            b.r = []
        self.ops.append(o)
        self.last[eng] = o
        if o.is_dma:
            self.dmas.append(o)
        return o

    def barrier(self):
        deps = list(self.last.values()) + list(self.dmas)
        self.dmas = []
        for eng in self.ENGS:
            self.op(eng, lambda e: e.nop(), extra_deps=deps)

    def new_sem(self, name):
        self.nsem += 1
        return self.stack.enter_context(self.nc.semaphore(f"{name}_{self.nsem}"))

    def flush(self, final_wait_ops=()):
        LIM = 30000
        for o in final_wait_ops:
            o.needs_inc = True
        for b in self.bufs:
            if b.w is not None:
                b.w.needs_inc = True
            for r in b.r:
                r.needs_inc = True
        for o in self.last.values():
            o.needs_inc = True
        for o in self.ops:
            e = self.eng_obj[o.eng]
            need = {}
            for d in o.deps:
                sem, val = d.ev
                k = id(sem)
                if k not in need or need[k][1] < val:
                    need[k] = (sem, val)
            w = self.waited[o.eng]
            for k, (sem, val) in need.items():
                cur = w.get(k)
                if cur is None or cur < val:
                    e.wait_ge(sem, val)
                    w[k] = val
            ins = o.fn(e)
            if o.is_dma:
                t = (o.eng, o.tag)
                if t not in self.tag_sem or self.tag_cnt[t] + 16 > LIM:
                    self.tag_sem[t] = self.new_sem("d" + o.eng + str(o.tag))
                    self.tag_cnt[t] = 0
                self.tag_cnt[t] += 16
                ins.then_inc(self.tag_sem[t], 16)
                o.ev = (self.tag_sem[t], self.tag_cnt[t])
            elif o.needs_inc:
                if o.eng not in self.eng_sem or self.eng_cnt[o.eng] + 1 > LIM:
                    self.eng_sem[o.eng] = self.new_sem("e" + o.eng)
                    self.eng_cnt[o.eng] = 0
                self.eng_cnt[o.eng] += 1
                ins.then_inc(self.eng_sem[o.eng], 1)
                o.ev = (self.eng_sem[o.eng], self.eng_cnt[o.eng])
            o.fn = None
        self.ops = []
        fw = {}
        for o in final_wait_ops:
            sem, val = o.ev
            if id(sem) not in fw or fw[id(sem)][1] < val:
                fw[id(sem)] = (sem, val)
        for sem, val in fw.values():
            self.nc.sync.wait_ge(sem, val)


class _Stop(Exception):
    pass


def V(base, extra, dims):
    return bass.AP(base.tensor, base.offset + extra, [list(base.ap[0])] + [list(d) for d in dims])


def build(nl=NL, dbg=(), stop_after=None, ngroups=8):
    nc = bass.Bass("TRN2", target_bir_lowering=False)

    def din(name, shape):
        return nc.dram_tensor(name, shape, F32, kind="ExternalInput").ap()

    xT_d = din("xT", [1024, 2048])
    cT_d = din("cT", [128, 8])
    wada_d = din("w_ada", [NL, 1024, 6144])
    bada_d = din("b_adaT", [NL, 128, 48])
    gains_d = din("gains", [NL, 128, 32])
    sinks_d = din("sinks", [NL, 128, 8])
    win_d = din("w_in_c", [NL, 11, 128, 1024])
    poolw_d = din("pool_w", [NL, 4, 128, 128])
    wout_d = din("w_out_c", [NL, 8, 128, 1024])
    wq_d = din("wq_c", [NL, 16, 128, 1024])
    skT_d = din("skT", [NL, 2, 128, 128])
    uT_d = din("uT", [NL, 128, 128, 1024])
    v_d = din("peer_v", [NL, 16384, 1024])
    ident_d = din("ident", [128, 128])
    biasT_d = din("biasT", [2, 128, 1024])
    fix_d = din("poolfix", [128, 64])
    oT_d = nc.dram_tensor("oT", [1024, 2048], F32, kind="ExternalOutput").ap()
    cu_d = nc.dram_tensor("cache_u", [128, 128, 1024], BF16, kind="Internal").ap()
    cv_d = nc.dram_tensor("cache_v", [128, 128, 1024], BF16, kind="Internal").ap()
    dbg_d = {}
    for name, shape in dbg:
        dbg_d[name] = nc.dram_tensor("dbg_" + name, list(shape), F32, kind="ExternalOutput").ap()
    dbg_ops = []

    with ExitStack() as st:
        P = Prog(nc, st)

        uniq = [0]

        def sb(stack, name, shape, dt=F32):
            uniq[0] += 1
            return stack.enter_context(nc.sbuf_tensor(f"s{uniq[0]}_{name}", list(shape), dt))

        xT = sb(st, "xT", [128, 8, 2048])
        identf = sb(st, "identf", [128, 128])
        identb = sb(st, "identb", [128, 128], BF16)
        onesb = sb(st, "onesb", [128, 128], BF16)
        bdb = sb(st, "bdb", [128, 128], BF16)
        biasT = sb(st, "biasT", [128, 2, 1024])
        fix = sb(st, "fix", [128, 4, 16])
        cT = sb(st, "cT", [128, 8])
        condb = sb(st, "condb", [128, 8], BF16)
        modT = sb(st, "modT", [128, 48])
        badaT = sb(st, "badaT", [128, 48])
        gains = sb(st, "gains", [128, 32])
        esink = sb(st, "esink", [128, 8])
        a1 = sb(st, "a1", [128, 8])
        a2 = sb(st, "a2", [128, 8])
        qg8 = sb(st, "qg8", [128, 1])
        ps = st.enter_context(nc.psum_tensor("ps", [128, 8, 512], F32))

        BX = [[P.buf(f"x{k}_{g}") for g in range(8)] for k in range(8)]
        PB = [P.buf(f"psb{b}") for b in range(8)]
        B_const = P.buf("const")
        B_mod = P.buf("mod")
        B_small = P.buf("small")
        B_dbg = P.buf("dbg")

        def bx(k, t0, t1):
            return [BX[k][g] for g in range(t0 // 256, (t1 + 255) // 256)]

        def dump(name, src_ap, reads):
            if name in dbg_d:
                dst = dbg_d[name]
                if len(src_ap.shape) == 3 and src_ap.shape[1] * src_ap.shape[2] > 4096:
                    for k in range(src_ap.shape[1]):
                        dbg_ops.append(P.op("pool", lambda e, k=k: e.dma_start(out=dst[:, k, :], in_=src_ap[:, k, :]), reads=reads, writes=[B_dbg], dma_tag="dbg"))
                else:
                    dbg_ops.append(P.op("pool", lambda e: e.dma_start(out=dst, in_=src_ap), reads=reads, writes=[B_dbg], dma_tag="dbg"))

        for k in range(8):
            P.op("sp", lambda e, k=k: e.dma_start(out=xT[:, k, :], in_=xT_d[k * 128:(k + 1) * 128, :]),
                 writes=BX[k], dma_tag=f"x{k}")
        P.op("sp", lambda e: e.dma_start(out=identf[:], in_=ident_d), writes=[B_const], dma_tag="cc")
        P.op("pool", lambda e: e.dma_start(out=identb[:], in_=ident_d), writes=[B_const], dma_tag="cc")
        P.op("sp", lambda e: e.dma_start(out=biasT[:], in_=biasT_d.rearrange("k p n -> p k n")), writes=[B_const], dma_tag="cc")
        P.op("sp", lambda e: e.dma_start(out=fix[:].rearrange("p g n -> p (g n)"), in_=fix_d), writes=[B_const], dma_tag="cc")
        P.op("sp", lambda e: e.dma_start(out=cT[:], in_=cT_d), writes=[B_const], dma_tag="cc")
        P.op("dve", lambda e: e.memset(onesb[:], 1.0), writes=[B_const])
        P.op("dve", lambda e: e.memset(bdb[:], 0.0), writes=[B_const])
        P.op("dve", lambda e: e.memset(bdb[0:64, 0:64], 1.0), writes=[B_const])
        P.op("dve", lambda e: e.memset(bdb[64:128, 64:128], 1.0), writes=[B_const])
        P.op("act", lambda e: e.activation(out=condb[:], in_=cT[:], func=AF.Silu), reads=[B_const], writes=[B_const])

        def norm_mod(stk, xlo, n, a_t, sh_col0, out_ap_fn, out_bufs_fn, bank, tmp_tag):
            sqb = [sb(stk, f"sqb{tmp_tag}{i}", [128, n], BF16) for i in range(2)]
            rs = sb(stk, f"rs{tmp_tag}", [128, n])
            tmp = [sb(stk, f"nt{tmp_tag}{i}", [128, n]) for i in range(2)]
            Bsq = [P.buf() for _ in range(2)]
            Brs = P.buf()
            Btmp = [P.buf() for _ in range(2)]

            def run(t0):
                for k in range(8):
                    P.op("act", lambda e, k=k: e.activation(out=sqb[k % 2][:], in_=xT[:, k, t0:t0 + n], func=AF.Square),
                         reads=bx(k, t0, t0 + n), writes=[Bsq[k % 2]])
                    P.op("pe", lambda e, k=k: e.matmul(ps[:, bank, 0:n], lhsT=onesb[:], rhs=sqb[k % 2][:], start=(k == 0), stop=(k == 7)),
                         reads=[Bsq[k % 2], B_const], writes=[PB[bank]])
                P.op("act", lambda e: e.activation(out=rs[:], in_=ps[:, bank, 0:n], func=AF.Ln, scale=1.0 / D, bias=EPS),
                     reads=[PB[bank]], writes=[Brs])
                P.op("act", lambda e: e.activation(out=rs[:], in_=rs[:], func=AF.Exp, scale=-0.5), reads=[Brs], writes=[Brs])
                for k in range(8):
                    P.op("dve", lambda e, k=k: e.scalar_tensor_tensor(out=tmp[k % 2][:], in0=xT[:, k, t0:t0 + n], scalar=a_t[:, k:k + 1],
                                                                      in1=rs[:], op0=ALU.mult, op1=ALU.mult),
                         reads=bx(k, t0, t0 + n) + [Brs, B_mod], writes=[Btmp[k % 2]])
                    P.op("pool", lambda e, k=k: e.tensor_scalar(out=out_ap_fn(k, t0), in0=tmp[k % 2][:], scalar1=modT[:, sh_col0 + k:sh_col0 + k + 1],
                                                                scalar2=None, op0=ALU.add),
                         reads=[Btmp[k % 2], B_mod], writes=out_bufs_fn(k, t0))
            return run

        stopped = [False]

        def chk(name, l):
            if stop_after == (name, l):
                P.barrier()
                P.flush()
                raise _Stop()

        try:
          for l in range(nl):
              with ExitStack() as sa:
                  wada = [sb(sa, f"wada{i}", [128, 8, 1024], BF16) for i in range(2)]
                  Bw = [P.buf() for _ in range(2)]
                  P.op("sp", lambda e, l=l: e.dma_start(out=badaT[:], in_=bada_d[l]), writes=[B_mod], dma_tag="cm")
                  P.op("sp", lambda e, l=l: e.dma_start(out=gains[:], in_=gains_d[l]), writes=[B_mod], dma_tag="cm")
                  P.op("sp", lambda e, l=l: e.dma_start(out=esink[:], in_=sinks_d[l]), writes=[B_mod], dma_tag="cm")
                  for m in range(6):
                      P.op("pool", lambda e, m=m, l=l: e.dma_start(
                          out=wada[m % 2][:], in_=wada_d[l][:, m * 1024:(m + 1) * 1024].rearrange("(k p) n -> p k n", p=128)),
                          writes=[Bw[m % 2]], dma_tag=f"wa{m % 2}")

                      def mm_ada(e, m=m):
                          ins = None
                          for j in range(8):
                              for k in range(8):
                                  ins = e.matmul(ps[:, 0, m * 8 + j:m * 8 + j + 1], lhsT=wada[m % 2][:, k, j * 128:(j + 1) * 128],
                                                 rhs=condb[:, k:k + 1], start=(k == 0), stop=(k == 7))
                          return ins
                      P.op("pe", mm_ada, reads=[Bw[m % 2], B_const], writes=[PB[0]])
                  P.op("dve", lambda e: e.tensor_tensor(out=modT[:], in0=ps[:, 0, 0:48], in1=badaT[:], op=ALU.add),
                       reads=[PB[0], B_mod], writes=[B_mod])
                  P.op("dve", lambda e: e.scalar_tensor_tensor(out=a1[:], in0=modT[:, 8:16], scalar=1.0, in1=gains[:, 0:8], op0=ALU.add, op1=ALU.mult),
                       reads=[B_mod], writes=[B_mod])
                  P.op("dve", lambda e: e.scalar_tensor_tensor(out=a2[:], in0=modT[:, 32:40], scalar=1.0, in1=gains[:, 8:16], op0=ALU.add, op1=ALU.mult),
                       reads=[B_mod], writes=[B_mod])
                  P.op("act", lambda e: e.activation(out=esink[:], in_=esink[:], func=AF.Exp), reads=[B_mod], writes=[B_mod])
                  P.op("dve", lambda e: e.tensor_scalar(out=qg8[:], in0=gains[:, 28:29], scalar1=0.125, scalar2=None, op0=ALU.mult),
                       reads=[B_mod], writes=[B_mod])
                  dump(f"mod{l}", modT[:], [B_mod])
                  P.barrier()
                  P.flush()
                  P.drop(Bw)
              if stop_after == ("ada", l):
                  break

              with ExitStack() as s1:
                  try:
                      hT = sb(s1, "hT", [128, 8, 2048], BF16)
                      BH = [[P.buf() for _ in range(4)] for _ in range(8)]
                      ypr = sb(s1, "ypr", [128, 4, 2048], BF16)
                      BYP = [[P.buf() for _ in range(4)] for _ in range(4)]
                      pT = sb(s1, "pT", [128, 2048])
                      tA = sb(s1, "tA", [128, 2048])
                      tB = sb(s1, "tB", [128, 2048])
                      pooled = sb(s1, "pooled", [128, 2048], BF16)
                      B_pT, B_tA, B_tB, B_pooled = P.buf(), P.buf(), P.buf(), P.buf()
                      qnT = sb(s1, "qnT", [128, 4, 2048], BF16)
                      knT = sb(s1, "knT", [128, 2, 2048], BF16)
                      BQ = [[P.buf() for _ in range(4)] for _ in range(4)]
                      BK = [[P.buf() for _ in range(4)] for _ in range(2)]
                      v_sb = sb(s1, "v_sb", [128, 16, 2, 65], BF16)
                      BV = [P.buf() for _ in range(16)]
                      wch = [sb(s1, f"wch{i}", [128, 8, 128], BF16) for i in range(2)]
                      BW = [P.buf() for _ in range(2)]
                      poolw = sb(s1, "poolw", [128, 4, 128], BF16)
                      B_pw = P.buf()
                      local_bufs = [b for row in BH for b in row] + [b for row in BYP for b in row] + [B_pT, B_tA, B_tB, B_pooled] + \
                          [b for row in BQ for b in row] + [b for row in BK for b in row] + BV + BW + [B_pw]
                      wslot = [0]

                      def load_w(src_ap):
                          i = wslot[0] % 2
                          wslot[0] += 1
                          P.op("pool", lambda e: e.dma_start(out=wch[i][:].rearrange("p k n -> p (k n)"), in_=src_ap), writes=[BW[i]], dma_tag=f"w{i}")
                          return i

                      P.op("pool", lambda e, l=l: e.dma_start(out=poolw[:], in_=poolw_d[l].rearrange("g c d -> c g d")), writes=[B_pw], dma_tag="pw")
                      P.op("dve", lambda e: e.memset(v_sb[:, :, :, 64:65], 1.0), writes=BV)

                      nm = norm_mod(s1, 0, 512, a1, 0, lambda k, t0: hT[:, k, t0:t0 + 512], lambda k, t0: [BH[k][t0 // 512]], 1, "a")
                      for tg in range(4):
                          nm(tg * 512)

                      dump(f"h{l}", hT[:], [b for row in BH for b in row])
                      chk("norm", l)
                      qraw = sb(s1, "qraw", [128, 512])
                      qsq = sb(s1, "qsq", [128, 512], BF16)
                      qrs = sb(s1, "qrs", [128, 512])
                      B_qraw, B_qsq, B_qrs = P.buf(), P.buf(), P.buf()
                      local_bufs += [B_qraw, B_qsq, B_qrs]
                      pbank = [0]
                      for oc in range(4, 10):
                          wi = load_w(win_d[l, oc])
                          for tg in range(4):
                              bk = 2 + (pbank[0] % 2)
                              pbank[0] += 1

                              def mmq(e, wi=wi, tg=tg, bk=bk):
                                  ins = None
                                  for k in range(8):
                                      ins = e.matmul(ps[:, bk, :], lhsT=wch[wi][:, k, :], rhs=hT[:, k, tg * 512:(tg + 1) * 512], start=(k == 0), stop=(k == 7))
                                  return ins
                              P.op("pe", mmq, reads=[BW[wi]] + [BH[k][tg] for k in range(8)], writes=[PB[bk]])
                              P.op("act", lambda e, bk=bk: e.activation(out=qraw[:], in_=ps[:, bk, :], func=AF.Copy), reads=[PB[bk]], writes=[B_qraw])
                              P.op("act", lambda e: e.activation(out=qsq[:], in_=qraw[:], func=AF.Square), reads=[B_qraw], writes=[B_qsq])
                              P.op("pe", lambda e: e.matmul(ps[:, 1, :], lhsT=bdb[:], rhs=qsq[:], start=True, stop=True), reads=[B_qsq, B_const], writes=[PB[1]])
                              P.op("act", lambda e: e.activation(out=qrs[:], in_=ps[:, 1, :], func=AF.Ln, scale=1.0 / 64, bias=EPS), reads=[PB[1]], writes=[B_qrs])
                              P.op("act", lambda e: e.activation(out=qrs[:], in_=qrs[:], func=AF.Exp, scale=-0.5), reads=[B_qrs], writes=[B_qrs])
                              if oc < 8:
                                  dst, dbuf, gcol = qnT[:, oc - 4, tg * 512:(tg + 1) * 512], BQ[oc - 4][tg], qg8[:, 0:1]
                              else:
                                  dst, dbuf, gcol = knT[:, oc - 8, tg * 512:(tg + 1) * 512], BK[oc - 8][tg], gains[:, 29:30]
                              P.op("dve", lambda e, dst=dst, gcol=gcol: e.scalar_tensor_tensor(out=dst, in0=qraw[:], scalar=gcol, in1=qrs[:], op0=ALU.mult, op1=ALU.mult),
                                   reads=[B_qraw, B_qrs, B_mod], writes=[dbuf])

                      dump(f"qn{l}", qnT[:], [b for row in BQ for b in row])
                      dump(f"kn{l}", knT[:], [b for row in BK for b in row])
                      chk("qk", l)
                      wi = load_w(win_d[l, 10])
                      for tt in range(16):
                          bk = 2 + (pbank[0] % 2)
                          pbank[0] += 1

                          def mmv(e, wi=wi, tt=tt, bk=bk):
                              ins = None
                              for k in range(8):
                                  ins = e.matmul(ps[:, bk, 0:128], lhsT=hT[:, k, tt * 128:(tt + 1) * 128], rhs=wch[wi][:, k, :], start=(k == 0), stop=(k == 7))
                              return ins
                          P.op("pe", mmv, reads=[BW[wi]] + [BH[k][tt // 4] for k in range(8)], writes=[PB[bk]])
                          P.op("act", lambda e, tt=tt, bk=bk: e.activation(out=v_sb[:, tt, :, 0:64], in_=ps[:, bk, 0:128].rearrange("p (a b) -> p a b", a=2), func=AF.Copy),
                               reads=[PB[bk]], writes=[BV[tt]])

                      dump(f"v{l}", v_sb[:], BV)
                      chk("v", l)
                      for g in range(4):
                          w = (2, 4, 8, 16)[g]
                          wi = load_w(win_d[l, g])
                          for tg in range(4):
                              bk = 2 + (pbank[0] % 2)
                              pbank[0] += 1

                              def mmp(e, wi=wi, tg=tg, bk=bk):
                                  ins = None
                                  for k in range(8):
                                      ins = e.matmul(ps[:, bk, :], lhsT=wch[wi][:, k, :], rhs=hT[:, k, tg * 512:(tg + 1) * 512], start=(k == 0), stop=(k == 7))
                                  return ins
                              P.op("pe", mmp, reads=[BW[wi]] + [BH[k][tg] for k in range(8)], writes=[PB[bk]])
                              P.op("act", lambda e, tg=tg, bk=bk: e.activation(out=pT[:, tg * 512:(tg + 1) * 512], in_=ps[:, bk, :], func=AF.Copy), reads=[PB[bk]], writes=[B_pT])
                          src, Bsrc = pT, B_pT
                          bufs2 = [(tA, B_tA), (tB, B_tB)]
                          sh = 1
                          step = 0
                          while sh < w:
                              dst, Bdst = bufs2[step % 2]
                              eng = "dve" if step % 2 == 0 else "pool"
                              P.op(eng, lambda e, dst=dst, src=src, sh=sh: e.tensor_tensor(out=dst[:, sh:], in0=src[:, sh:], in1=src[:, :S - sh], op=ALU.add),
                                   reads=[Bsrc], writes=[Bdst])
                              P.op(eng, lambda e, dst=dst, src=src, sh=sh: e.tensor_copy(out=dst[:, 0:sh], in_=src[:, 0:sh]), reads=[Bsrc], writes=[Bdst])
                              src, Bsrc = dst, Bdst
                              sh *= 2
                              step += 1
                          P.op("dve", lambda e, src=src, g=g: e.tensor_tensor(out=src[:, 0:16], in0=src[:, 0:16], in1=fix[:, g, :], op=ALU.mult),
                               reads=[Bsrc, B_const], writes=[Bsrc])
                          P.op("dve", lambda e, src=src, w=w: e.scalar_tensor_tensor(out=pooled[:], in0=src[:], scalar=1.0 / w, in1=pT[:], op0=ALU.mult, op1=ALU.subtract),
                               reads=[Bsrc, B_pT], writes=[B_pooled])
                          for tg in range(4):
                              bk = 2 + (pbank[0] % 2)
                              pbank[0] += 1
                              P.op("pe", lambda e, g=g, tg=tg, bk=bk: e.matmul(ps[:, bk, :], lhsT=poolw[:, g, :], rhs=pooled[:, tg * 512:(tg + 1) * 512], start=True, stop=True),
                                   reads=[B_pw, B_pooled], writes=[PB[bk]])
                              P.op("act", lambda e, g=g, tg=tg, bk=bk: e.activation(out=ypr[:, g, tg * 512:(tg + 1) * 512], in_=ps[:, bk, :], func=AF.Identity,
                                                                                 scale=gains[:, 24 + g:25 + g]),
                                   reads=[PB[bk], B_mod], writes=[BYP[g][tg]])

                      dump(f"ypr{l}", ypr[:], [b for row in BYP for b in row])
                      chk("pool", l)
                      prs, B_prs = qrs, B_qrs
                      for tg in range(4):
                          for g in range(4):
                              P.op("act", lambda e, g=g, tg=tg: e.activation(out=qsq[:], in_=ypr[:, g, tg * 512:(tg + 1) * 512], func=AF.Square),
                                   reads=[BYP[g][tg]], writes=[B_qsq])
                              P.op("pe", lambda e, g=g: e.matmul(ps[:, 1, :], lhsT=onesb[:], rhs=qsq[:], start=(g == 0), stop=(g == 3)),
                                   reads=[B_qsq, B_const], writes=[PB[1]])
                          P.op("act", lambda e: e.activation(out=prs[:], in_=ps[:, 1, :], func=AF.Ln, scale=1.0 / 512, bias=EPS), reads=[PB[1]], writes=[B_prs])
                          P.op("act", lambda e: e.activation(out=prs[:], in_=prs[:], func=AF.Exp, scale=-0.5), reads=[B_prs], writes=[B_prs])
                          for g in range(4):
                              P.op("dve", lambda e, g=g, tg=tg: e.scalar_tensor_tensor(out=hT[:, g, tg * 512:(tg + 1) * 512], in0=ypr[:, g, tg * 512:(tg + 1) * 512],
                                                                                   scalar=gains[:, 16 + g:17 + g], in1=prs[:], op0=ALU.mult, op1=ALU.mult),
                                   reads=[BYP[g][tg], B_prs, B_mod], writes=[BH[g][tg]])

                      chk("poolnorm", l)
                      ssb = sb(s1, "ssb", [128, 1024])
                      PT = sb(s1, "PT", [128, 1024], BF16)
                      B_ssb, B_PT = P.buf(), P.buf()
                      den = sb(s1, "den", [128, 8])
                      yatt = sb(s1, "yatt", [128, 8, 64])
                      ssq = sb(s1, "ssq", [128, 2])
                      yab = sb(s1, "yab", [128, 512], BF16)
                      B_den, B_yatt, B_ssq, B_yab = P.buf(), P.buf(), P.buf(), P.buf()
                      yjunk, B_yjunk = ssb, B_ssb
                      local_bufs += [B_ssb, B_PT, B_den, B_yatt, B_ssq, B_yab]
                      for n in range(16):
                          js = [1] if n == 0 else [0, 1]
                          for kh in range(2):
                              def mms(e, n=n, kh=kh):
                                  ins = None
                                  for j in range(2):
                                      kb = max(n - 1 + j, 0)
                                      for g in range(4):
                                          h = kh * 4 + g
                                          r = h % 2
                                          lo = r * 64
                                          col = (j * 2 + g // 2) * 128
                                          ins = e.matmul(ps[:, 4 + r, col:col + 128], lhsT=knT[lo:lo + 64, kh, kb * 128:(kb + 1) * 128],
                                                         rhs=qnT[lo:lo + 64, h // 2, n * 128:(n + 1) * 128], start=True, stop=True)
                                  return ins
                              rd = [BK[kh][(n - 1) // 4 if n > 0 else 0], BK[kh][n // 4], BQ[kh * 2][n // 4], BQ[kh * 2 + 1][n // 4]]
                              P.op("pe", mms, reads=rd, writes=[PB[4], PB[5]])
                              P.op("dve", lambda e, kh=kh: e.tensor_tensor(out=ssb[:], in0=V(ps[:, 4, :], 0, [[1, 1024]]),
                                                                          in1=biasT[:, kh, :], op=ALU.add),
                                   reads=[PB[4], PB[5], B_const], writes=[B_ssb])
                              P.op("act", lambda e: e.activation(out=PT[:], in_=ssb[:], func=AF.Exp), reads=[B_ssb], writes=[B_PT])

                              def mmo(e, n=n, kh=kh, js=js):
                                  ins = None
                                  for g in range(4):
                                      for j in js:
                                          kb = n - 1 + j
                                          col = (g % 2) * 512 + (j * 2 + g // 2) * 128
                                          ins = e.matmul(ps[:, 6 + kh, g * 65:(g + 1) * 65], lhsT=PT[:, col:col + 128],
                                                         rhs=v_sb[:, kb, kh, :], start=(j == js[0]), stop=(j == 1))
                                  return ins
                              P.op("pe", mmo, reads=[B_PT, BV[max(n - 1, 0)], BV[n]], writes=[PB[6 + kh]])
                          for kh in range(2):
                              P.op("dve", lambda e, kh=kh: e.tensor_tensor(out=den[:, kh * 4:(kh + 1) * 4], in0=V(ps[:, 6 + kh, :], 64, [[65, 4]]),
                                                                          in1=esink[:, kh * 4:(kh + 1) * 4], op=ALU.add),
                                   reads=[PB[6 + kh], B_mod], writes=[B_den])
                          P.op("dve", lambda e: e.reciprocal(out=den[:], in_=den[:]), reads=[B_den], writes=[B_den])
                          for kh in range(2):
                              P.op("dve", lambda e, kh=kh: e.tensor_tensor(out=yatt[:, kh * 4:(kh + 1) * 4, :], in0=V(ps[:, 6 + kh, :], 0, [[65, 4], [1, 64]]),
                                                                          in1=V(den[:], kh * 4, [[1, 4], [0, 64]]), op=ALU.mult),
                                   reads=[PB[6 + kh], B_den], writes=[B_yatt])
                          P.op("act", lambda e: e.activation(out=yjunk[:, 0:512], in_=yatt[:].rearrange("p a b -> p (a b)"), func=AF.Square, accum_out=ssq[:, 0:1]),
                               reads=[B_yatt], writes=[B_ssq, B_yjunk])
                          P.op("act", lambda e: e.activation(out=ssq[:, 1:2], in_=ssq[:, 0:1], func=AF.Ln, scale=1.0 / 512, bias=EPS), reads=[B_ssq], writes=[B_ssq])
                          P.op("act", lambda e: e.activation(out=ssq[:, 1:2], in_=ssq[:, 1:2], func=AF.Exp, scale=-0.5), reads=[B_ssq], writes=[B_ssq])
                          P.op("dve", lambda e: e.tensor_scalar(out=yab[:], in0=yatt[:].rearrange("p a b -> p (a b)"), scalar1=ssq[:, 1:2], scalar2=None, op0=ALU.mult),
                               reads=[B_yatt, B_ssq], writes=[B_yab])
                          psb = ps[:, 1, :].bitcast(BF16)

                          def trn(e):
                              ins = None
                              for c in range(4):
                                  ins = e.transpose(psb[:, c * 128:(c + 1) * 128], yab[:, c * 128:(c + 1) * 128], identb[:])
                              return ins
                          P.op("pe", trn, reads=[B_yab, B_const], writes=[PB[1]])
                          for c in range(4):
                              P.op("act", lambda e, c=c, n=n: e.activation(out=hT[:, 4 + c, n * 128:(n + 1) * 128], in_=psb[:, c * 128:(c + 1) * 128], func=AF.Identity,
                                                                        scale=gains[:, 20 + c:21 + c]),
                                   reads=[PB[1], B_mod], writes=[BH[4 + c][n // 4]])
                      dump(f"y{l}", hT[:], [b for row in BH for b in row])

                      chk("attn", l)
                      for oc in range(8):
                          wi = load_w(wout_d[l, oc])
                          for tg in range(4):
                              bk = 2 + (pbank[0] % 2)
                              pbank[0] += 1

                              def mmw(e, wi=wi, tg=tg, bk=bk):
                                  ins = None
                                  for k in range(8):
                                      ins = e.matmul(ps[:, bk, :], lhsT=wch[wi][:, k, :], rhs=hT[:, k, tg * 512:(tg + 1) * 512], start=(k == 0), stop=(k == 7))
                                  return ins
                              P.op("pe", mmw, reads=[BW[wi]] + [BH[k][tg] for k in range(8)], writes=[PB[bk]])
                              P.op("dve", lambda e, oc=oc, tg=tg, bk=bk: e.scalar_tensor_tensor(out=xT[:, oc, tg * 512:(tg + 1) * 512], in0=ps[:, bk, :],
                                                                                              scalar=modT[:, 16 + oc:17 + oc], in1=xT[:, oc, tg * 512:(tg + 1) * 512],
                                                                                              op0=ALU.mult, op1=ALU.add),
                                   reads=[PB[bk], B_mod] + bx(oc, tg * 512, (tg + 1) * 512), writes=bx(oc, tg * 512, (tg + 1) * 512))
                      dump(f"x1_{l}", xT[:], [b for row in BX for b in row])
                      P.barrier()
                      P.flush()
                      P.drop(local_bufs)
                  except _Stop:
                      stopped[0] = True
              if stopped[0]:
                  break
              if stop_after == ("s1", l):
                  break

              with ExitStack() as s2:
                  try:
                      GT = sb(s2, "GT", [128, 256, 128], BF16)
                      B_GT = P.buf()
                      h2g = sb(s2, "h2g", [128, 8, 256], BF16)
                      B_h2 = [P.buf() for _ in range(8)]
                      NSL = 7
                      reg = sb(s2, "reg", [128, NSL * 2048], BF16)
                      qTg = reg[:, 0:4096].rearrange("p (a t) -> p a t", a=16)
                      B_qT = [P.buf() for _ in range(16)]

                      def rv(s_, i):
                          b0 = 4096 + s_ * 3072 + i * 512
                          return reg[:, b0:b0 + 512].rearrange("p (t n) -> p t n", t=4)
                      qrep = [[rv(s_, 0), rv(s_, 1)] for s_ in range(2)]
                      EQ = [rv(s_, 2) for s_ in range(2)]
                      Lm = [rv(s_, 3) for s_ in range(2)]
                      E2 = [rv(s_, 4) for s_ in range(2)]
                      Rm = [rv(s_, 5) for s_ in range(2)]
                      B_qrep = [[P.buf(), P.buf()] for _ in range(2)]
                      B_EQ = [P.buf() for _ in range(2)]
                      B_L = [P.buf() for _ in range(2)]
                      B_E2 = [P.buf() for _ in range(2)]
                      B_R = [P.buf() for _ in range(2)]
                      ub = [reg[:, i * 2048:i * 2048 + 1024].rearrange("p (k n) -> p k n", k=8) for i in range(NSL)]
                      vb = [reg[:, i * 2048 + 1024:(i + 1) * 2048] for i in range(NSL)]
                      B_ub = [P.buf() for _ in range(NSL)]
                      B_vb = [P.buf() for _ in range(NSL)]
                      fdum = sb(s2, "fdum", [128, 2])
                      B_fd = P.buf()
                      region_bufs = B_qT + [b for r_ in B_qrep for b in r_] + B_EQ + B_L + B_E2 + B_R + B_ub + B_vb

                      def fence():
                          P.op("pool", lambda e: e.memset(fdum[:], 0.0), writes=region_bufs + [B_fd])

                      s_sb = sb(s2, "s_sb", [128, 16, 128])
                      wk = sb(s2, "wk", [128, 16, 128])
                      B_s, B_wk = P.buf(), P.buf()
                      top = sb(s2, "top", [128, 16, 16])
                      c16 = sb(s2, "c16", [128, 8, 16])
                      e16 = sb(s2, "e16", [128, 8, 16])
                      Zt = sb(s2, "Zt", [128, 8])
                      tme = sb(s2, "tme", [128, 8])
                      tok3 = sb(s2, "tok3", [128, 3, 128])
                      B_top, B_c16, B_e16, B_Z, B_tok3 = P.buf(), P.buf(), P.buf(), P.buf(), P.buf()
                      slotT = sb(s2, "slotT", [128, 3, 256])
                      B_slot = P.buf()
                      skb = sb(s2, "skb", [128, 2, 128], BF16)
                      B_sk = P.buf()
                      gl = [sb(s2, f"gl{i}", [128, 256]) for i in range(2)]
                      actT = [sb(s2, f"actT{i}", [128, 256], BF16) for i in range(2)]
                      B_gl = [P.buf() for _ in range(2)]
                      B_act = [P.buf() for _ in range(2)]
                      wqc = [sb(s2, f"wqc{i}", [128, 8, 128], BF16) for i in range(2)]
                      B_wq = [P.buf() for _ in range(2)]
                      B_cu = [P.buf() for _ in range(128)]
                      B_cv = [P.buf() for _ in range(128)]
                      local_bufs = [B_GT, B_s, B_wk, B_top, B_c16, B_e16, B_Z, B_tok3, B_slot, B_sk, B_fd] + \
                          B_h2 + region_bufs + B_gl + B_act + B_wq + B_cu + B_cv

                      P.op("pool", lambda e, l=l: e.dma_start(out=skb[:], in_=skT_d[l].rearrange("a d n -> d a n")), writes=[B_sk], dma_tag="sk")
                      nm2 = norm_mod(s2, 0, 256, a2, 24, lambda k, t0: h2g[:, k, :], lambda k, t0: [B_h2[k]], 7, "b")
                      cand = s_sb[:].rearrange("p a b -> p (a b)").rearrange("p (h n) -> p h n", h=8)
                      cwk = wk[:].rearrange("p a b -> p (a b)").rearrange("p (h n) -> p h n", h=8)

                      for G in range(ngroups):
                          t0 = G * 256
                          nm2(t0)
                          fence()
                          for hp in range(16):
                              i = hp % 2
                              P.op("pool", lambda e, i=i, hp=hp, l=l: e.dma_start(out=wqc[i][:].rearrange("p k n -> p (k n)"), in_=wq_d[l, hp]),
                                   writes=[B_wq[i]], dma_tag=f"wq{i}")
                              bk = 6 + (hp % 2)

                              def mmq2(e, i=i, bk=bk):
                                  ins = None
                                  for k in range(8):
                                      ins = e.matmul(ps[:, bk, 0:256], lhsT=wqc[i][:, k, :], rhs=h2g[:, k, :], start=(k == 0), stop=(k == 7))
                                  return ins
                              P.op("pe", mmq2, reads=[B_wq[i]] + B_h2, writes=[PB[bk]])
                              P.op("act", lambda e, hp=hp, bk=bk: e.activation(out=qTg[:, hp, :], in_=ps[:, bk, 0:256], func=AF.Copy), reads=[PB[bk]], writes=[B_qT[hp]])
                          for tt in range(2):
                              def mmsc(e, tt=tt):
                                  ins = None
                                  for hp in range(16):
                                      ins = e.matmul(ps[:, hp // 4, (hp % 4) * 128:(hp % 4 + 1) * 128], lhsT=qTg[:, hp, tt * 128:(tt + 1) * 128],
                                                     rhs=skb[:, hp % 2, :], start=True, stop=True)
                                  return ins
                              P.op("pe", mmsc, reads=B_qT + [B_sk], writes=PB[0:4])
                              for b4 in range(4):
                                  if b4 % 2 == 0:
                                      P.op("act", lambda e, b4=b4: e.activation(out=s_sb[:, b4 * 4:(b4 + 1) * 4, :].rearrange("p a b -> p (a b)"), in_=ps[:, b4, :], func=AF.Copy),
                                           reads=[PB[b4]], writes=[B_s])
                                  else:
                                      P.op("dve", lambda e, b4=b4: e.tensor_copy(out=s_sb[:, b4 * 4:(b4 + 1) * 4, :].rearrange("p a b -> p (a b)"), in_=ps[:, b4, :]),
                                           reads=[PB[b4]], writes=[B_s])
                              for hp in range(16):
                                  P.op("dve", lambda e, hp=hp: e.max(out=top[:, hp, 0:8], in_=s_sb[:, hp, :]), reads=[B_s], writes=[B_top])
                                  P.op("dve", lambda e, hp=hp: e.match_replace(out=wk[:, hp, :], in_to_replace=top[:, hp, 0:8], in_values=s_sb[:, hp, :], imm_value=-1e30),
                                       reads=[B_s, B_top], writes=[B_wk])
                                  P.op("dve", lambda e, hp=hp: e.max(out=top[:, hp, 8:16], in_=wk[:, hp, :]), reads=[B_wk], writes=[B_top])
                              P.op("dve", lambda e: e.tensor_tensor(out=cand.rearrange("p h (a b) -> p h a b", a=16),
                                                                    in0=V(top[:], 0, [[32, 8], [1, 16], [0, 16]]),
                                                                    in1=V(top[:], 16, [[32, 8], [0, 16], [1, 16]]), op=ALU.add),
                                   reads=[B_top], writes=[B_s])
                              for h in range(8):
                                  P.op("dve", lambda e, h=h: e.max(out=c16[:, h, 0:8], in_=cand[:, h, :]), reads=[B_s], writes=[B_c16])
                                  P.op("dve", lambda e, h=h: e.match_replace(out=cwk[:, h, :], in_to_replace=c16[:, h, 0:8], in_values=cand[:, h, :], imm_value=-1e30),
                                       reads=[B_s, B_c16], writes=[B_wk])
                                  P.op("dve", lambda e, h=h: e.max(out=c16[:, h, 8:16], in_=cwk[:, h, :]), reads=[B_wk], writes=[B_c16])
                              P.op("dve", lambda e: e.tensor_tensor(out=e16[:], in0=c16[:], in1=V(c16[:], 0, [[16, 8], [0, 16]]), op=ALU.subtract),
                                   reads=[B_c16], writes=[B_e16])
                              P.op("act", lambda e: e.activation(out=e16[:], in_=e16[:], func=AF.Exp), reads=[B_e16], writes=[B_e16])
                              P.op("dve", lambda e: e.tensor_reduce(out=Zt[:], in_=e16[:], axis=AX.X, op=ALU.add), reads=[B_e16], writes=[B_Z])
                              P.op("dve", lambda e: e.reciprocal(out=Zt[:], in_=Zt[:]), reads=[B_Z], writes=[B_Z])
                              P.op("dve", lambda e: e.tensor_scalar(out=tme[:], in0=V(c16[:], 15, [[16, 8]]), scalar1=-THR_EPS, scalar2=None, op0=ALU.add),
                                   reads=[B_c16], writes=[B_Z])
                              top1v = V(top[:], 0, [[32, 8], [1, 16]])
                              P.op("dve", lambda e: e.tensor_copy(out=tok3[:, 0, :].rearrange("p (h a) -> p h a", h=8), in_=top1v), reads=[B_top], writes=[B_tok3])
                              P.op("dve", lambda e: e.tensor_tensor(out=tok3[:, 1, :].rearrange("p (h a) -> p h a", h=8), in0=V(tme[:], 0, [[1, 8], [0, 16]]),
                                                                    in1=top1v, op=ALU.subtract), reads=[B_top, B_Z], writes=[B_tok3])
                              P.op("dve", lambda e: e.tensor_tensor(out=e16[:], in0=top1v, in1=V(c16[:], 0, [[16, 8], [0, 16]]), op=ALU.subtract),
                                   reads=[B_top, B_c16], writes=[B_e16])
                              P.op("act", lambda e: e.activation(out=e16[:], in_=e16[:], func=AF.Exp), reads=[B_e16], writes=[B_e16])
                              P.op("dve", lambda e: e.tensor_tensor(out=tok3[:, 2, :].rearrange("p (h a) -> p h a", h=8), in0=e16[:], in1=V(Zt[:], 0, [[1, 8], [0, 16]]), op=ALU.mult),
                                   reads=[B_e16, B_Z], writes=[B_tok3])

                              def trs(e):
                                  ins = None
                                  for i in range(3):
                                      ins = e.transpose(ps[:, 4, i * 128:(i + 1) * 128], tok3[:, i, :], identf[:])
                                  return ins
                              P.op("pe", trs, reads=[B_tok3, B_const], writes=[PB[4]])
                              P.op("act", lambda e, tt=tt: e.activation(out=slotT[:, :, tt * 128:(tt + 1) * 128], in_=ps[:, 4, 0:384].rearrange("p (a b) -> p a b", a=3), func=AF.Copy),
                                   reads=[PB[4]], writes=[B_slot])
                          if G == 0:
                              dump(f"slotT{l}", slotT[:], [B_slot])
                          NSB = 64

                          def front(sbi):
                              tl = sbi * 4
                              s_ = sbi % 2
                              for p_ in range(2):
                                  if p_ == 0:
                                      P.op("act", lambda e, p_=p_: e.activation(out=qrep[s_][p_].rearrange("p t (h a) -> p t h a", h=8),
                                                                               in_=V(reg[:], p_ * 256 + tl, [[1, 4], [512, 8], [0, 16]]), func=AF.Copy),
                                           reads=B_qT, writes=[B_qrep[s_][p_]])
                                  else:
                                      P.op("pool", lambda e, p_=p_: e.tensor_copy(out=qrep[s_][p_].rearrange("p t (h a) -> p t h a", h=8),
                                                                                 in_=V(reg[:], p_ * 256 + tl, [[1, 4], [512, 8], [0, 16]])),
                                           reads=B_qT, writes=[B_qrep[s_][p_]])

                                  def mmrep(e, p_=p_):
                                      ins = None
                                      for i in range(4):
                                          ins = e.matmul(ps[:, 2 * p_ + s_, i * 128:(i + 1) * 128], lhsT=qrep[s_][p_][:, i, :], rhs=skb[:, p_, :], start=True, stop=True)
                                      return ins
                                  P.op("pe", mmrep, reads=[B_qrep[s_][p_], B_sk], writes=[PB[2 * p_ + s_]])
                              ps1 = ps[:, s_, :].rearrange("p (t n) -> p t n", t=4)
                              ps2 = ps[:, 2 + s_, :].rearrange("p (t n) -> p t n", t=4)
                              P.op("dve", lambda e: e.tensor_tensor(out=EQ[s_], in0=ps1, in1=V(slotT[:], 0 * 256 + tl, [[1, 4], [0, 128]]), op=ALU.is_equal),
                                   reads=[PB[s_], B_slot], writes=[B_EQ[s_]])
                              P.op("act", lambda e: e.activation(out=E2[s_], in_=ps2, func=AF.Exp), reads=[PB[2 + s_]], writes=[B_E2[s_]])
                              P.op("dve", lambda e: e.tensor_tensor(out=Lm[s_], in0=EQ[s_], in1=V(slotT[:], 2 * 256 + tl, [[1, 4], [0, 128]]), op=ALU.mult),
                                   reads=[B_EQ[s_], B_slot], writes=[B_L[s_]])
                              P.op("dve", lambda e: e.tensor_tensor(out=EQ[s_], in0=ps2, in1=V(slotT[:], 1 * 256 + tl, [[1, 4], [0, 128]]), op=ALU.is_ge),
                                   reads=[PB[2 + s_], B_slot, B_E2[s_]], writes=[B_EQ[s_]])
                              P.op("dve", lambda e: e.tensor_tensor(out=Rm[s_], in0=EQ[s_], in1=E2[s_], op=ALU.mult), reads=[B_EQ[s_], B_E2[s_]], writes=[B_R[s_]])

                          def back(sbi):
                              tl = sbi * 4
                              s_ = sbi % 2

                              def mmg(e):
                                  ins = None
                                  for i in range(4):
                                      ins = e.matmul(ps[:, 4 + s_, i * 128:(i + 1) * 128], lhsT=Rm[s_][:, i, :], rhs=Lm[s_][:, i, :], start=True, stop=True)
                                  return ins
                              P.op("pe", mmg, reads=[B_R[s_], B_L[s_]], writes=[PB[4 + s_]])
                              P.op("act", lambda e: e.activation(out=GT[:, tl:tl + 4, :], in_=ps[:, 4 + s_, :].rearrange("p (t n) -> p t n", t=4), func=AF.Copy),
                                   reads=[PB[4 + s_]], writes=[B_GT])
                          for sbi in range(NSB + 1):
                              if sbi < NSB:
                                  front(sbi)
                              if sbi >= 1:
                                  back(sbi - 1)
                          if G == 0:
                              dump(f"GT{l}", GT[:], [B_GT])
                          fence()
                          def pfront(c):
                              sl = c % NSL
                              if G == 0:
                                  P.op("pool", lambda e: e.dma_start(out=ub[sl].rearrange("p k n -> p (k n)"), in_=uT_d[l, c]), writes=[B_ub[sl]], dma_tag=f"u{sl}")
                                  P.op("pool", lambda e: e.dma_start(out=vb[sl], in_=v_d[l, c * 128:(c + 1) * 128, :]), writes=[B_vb[sl]], dma_tag=f"v{sl}")
                                  P.op("sp", lambda e: e.dma_start(out=cu_d[c], in_=ub[sl].rearrange("p k n -> p (k n)")), reads=[B_ub[sl]], writes=[B_cu[c]], dma_tag=f"wu{sl}")
                                  P.op("sp", lambda e: e.dma_start(out=cv_d[c], in_=vb[sl]), reads=[B_vb[sl]], writes=[B_cv[c]], dma_tag=f"wv{sl}")
                              else:
                                  P.op("sp", lambda e: e.dma_start(out=ub[sl].rearrange("p k n -> p (k n)"), in_=cu_d[c]), reads=[B_cu[c]], writes=[B_ub[sl]], dma_tag=f"cu{sl}")
                                  P.op("sp", lambda e: e.dma_start(out=vb[sl], in_=cv_d[c]), reads=[B_cv[c]], writes=[B_vb[sl]], dma_tag=f"cv{sl}")
                              i2 = c % 2
                              bk = 6 + i2

                              def mma(e):
                                  ins = None
                                  for k in range(8):
                                      ins = e.matmul(ps[:, bk, 0:256], lhsT=ub[sl][:, k, :], rhs=h2g[:, k, :], start=(k == 0), stop=(k == 7))
                                  return ins
                              P.op("pe", mma, reads=[B_ub[sl]] + B_h2, writes=[PB[bk]])
                              P.op("act", lambda e: e.activation(out=gl[i2][:], in_=ps[:, bk, 0:256], func=AF.Gelu), reads=[PB[bk]], writes=[B_gl[i2]])
                              P.op("dve", lambda e: e.tensor_tensor(out=actT[i2][:], in0=gl[i2][:], in1=GT[:, :, c], op=ALU.mult),
                                   reads=[B_gl[i2], B_GT], writes=[B_act[i2]])

                          def pback(c):
                              sl = c % NSL
                              i2 = c % 2

                              def mmo2(e):
                                  ins = None
                                  for fk in range(8):
                                      ins = e.matmul(ps[:, fk // 2, (fk % 2) * 256:(fk % 2 + 1) * 256], lhsT=vb[sl][:, fk * 128:(fk + 1) * 128], rhs=actT[i2][:],
                                                     start=(c == 0 and fk % 2 == 0), stop=(c == 127), skip_group_check=True)
                                  return ins
                              P.op("pe", mmo2, reads=[B_vb[sl], B_act[i2]], writes=PB[0:4])
                          for c in range(129):
                              if c < 128:
                                  pfront(c)
                              if c >= 1:
                                  pback(c - 1)
                          for fk in range(8):
                              P.op("dve", lambda e, fk=fk, t0=t0: e.scalar_tensor_tensor(out=xT[:, fk, t0:t0 + 256], in0=ps[:, fk // 2, (fk % 2) * 256:(fk % 2 + 1) * 256],
                                                                                       scalar=modT[:, 40 + fk:41 + fk], in1=xT[:, fk, t0:t0 + 256], op0=ALU.mult, op1=ALU.add),
                                   reads=[PB[fk // 2], B_mod, BX[fk][G]], writes=[BX[fk][G]])
                          P.flush()
                      dump(f"x2_{l}", xT[:], [b for row in BX for b in row])
                      P.barrier()
                      P.flush()
                      P.drop(local_bufs)
                  except _Stop:
                      stopped[0] = True
              if stopped[0]:
                  break
        except _Stop:
            pass

        fin = list(dbg_ops)
        for k in range(8):
            fin.append(P.op("sp", lambda e, k=k: e.dma_start(out=oT_d[k * 128:(k + 1) * 128, :], in_=xT[:, k, :]), reads=BX[k], dma_tag="o"))
        P.flush(final_wait_ops=fin)
    return nc


def _consts():
    ident = np.eye(128, dtype=np.float32)
    slopes = (2.0 ** (-8.0 * np.arange(1, 9) / 8)).astype(np.float64)
    s = np.arange(128)[:, None]
    q = np.arange(128)[None, :]
    biasT = np.zeros((2, 128, 2, 2, 2, 128), np.float32)
    for kh in range(2):
        for g in range(4):
            sl = slopes[kh * 4 + g]
            d0 = q + 128 - s
            d1 = q - s
            biasT[kh, :, g % 2, 0, g // 2, :] = np.where((d0 >= 0) & (d0 < 128), -sl * d0, NEG)
            biasT[kh, :, g % 2, 1, g // 2, :] = np.where((d1 >= 0) & (d1 < 128), -sl * d1, NEG)
    fix = np.ones((128, 4, 16), np.float32)
    for g, w in enumerate((2, 4, 8, 16)):
        t = np.arange(16)
        fix[:, g, :] = (w / np.minimum(t + 1, w))[None, :]
    return ident, biasT.reshape(2, 128, 1024), fix.reshape(128, 64)


def prep_shared(inp):
    f = lambda a: np.ascontiguousarray(a, dtype=np.float32)
    sh = {}
    sh["w_ada"] = f(inp["w_ada"])
    sh["b_adaT"] = f(inp["b_ada"].reshape(NL, 6, 8, 128).transpose(0, 3, 1, 2).reshape(NL, 128, 48))
    gains = np.zeros((NL, 128, 32), np.float32)
    gains[:, :, 0:8] = inp["norm1_g"].reshape(NL, 8, 128).transpose(0, 2, 1)
    gains[:, :, 8:16] = inp["norm2_g"].reshape(NL, 8, 128).transpose(0, 2, 1)
    gains[:, :, 16:24] = inp["mix_norm_g"].reshape(NL, 8, 128).transpose(0, 2, 1)
    gains[:, :, 24:28] = inp["pool_scale"].reshape(NL, 4, 128).transpose(0, 2, 1)
    gains[:, :, 28] = np.tile(inp["q_norm_g"], (1, 2))
    gains[:, :, 29] = np.tile(inp["k_norm_g"], (1, 2))
    sh["gains"] = gains
    sh["sinks"] = f(np.broadcast_to(inp["attn_sinks"][:, None, :], (NL, 128, 8)))
    w_in = inp["w_in"]
    ext = np.concatenate([w_in[:, :, 0:1024], w_in[:, :, 1024:1088], w_in[:, :, 1024:1088], w_in[:, :, 1088:1152], w_in[:, :, 1088:1152],
                          w_in[:, :, 1152:1280]], axis=2)
    chunkify = lambda w, noc: f(w.reshape(NL, 8, 128, noc, 128).transpose(0, 3, 2, 1, 4).reshape(NL, noc, 128, 1024))
    sh["w_in_c"] = chunkify(ext, 11)
    sh["pool_w"] = f(inp["pool_w"])
    sh["w_out_c"] = chunkify(inp["w_out"], 8)
    sh["wq_c"] = chunkify(inp["peer_wq"], 16)
    sh["skT"] = f(inp["peer_subkeys"].transpose(0, 1, 3, 2))
    sh["uT"] = f(inp["peer_u"].reshape(NL, 128, 128, 8, 128).transpose(0, 1, 4, 3, 2).reshape(NL, 128, 128, 1024))
    sh["peer_v"] = f(inp["peer_v"])
    ident, biasT, fix = _consts()
    sh["ident"] = ident
    sh["biasT"] = biasT
    sh["poolfix"] = fix
    return sh


def prep_core(inp, b):
    return {"xT": np.ascontiguousarray(inp["x"][b].T, dtype=np.float32),
            "cT": np.ascontiguousarray(inp["c"][b].reshape(8, 128).T, dtype=np.float32)}


def kernel(**inputs):
    inp = {k: np.asarray(v) for k, v in inputs.items()}
    nb = inp["x"].shape[0]
    sh = prep_shared(inp)
    in_maps = []
    for b in range(nb):
        m = dict(sh)
        m.update(prep_core(inp, b))
        in_maps.append(m)
    nc = build()
    res = run_bass_kernel_spmd(nc, in_maps, core_ids=list(range(nb)))
    out = np.stack([np.asarray(res.results[b]["oT"]).T for b in range(nb)], axis=0)
    return np.ascontiguousarray(out.astype(np.float32))
```

```python
import numpy as np
from contextlib import ExitStack
import concourse.bass as bass
import concourse.mybir as mybir
from concourse.bass_utils import run_bass_kernel_spmd

F32 = mybir.dt.float32
BF16 = mybir.dt.bfloat16
AF = mybir.ActivationFunctionType
ALU = mybir.AluOpType
AX = mybir.AxisListType

NL = 4
S = 2048
D = 1024
EPS = 1e-6
NEG = -30000.0
THR_EPS = 4e-6


class Buf:
    __slots__ = ("name", "w", "r")

    def __init__(self, name=""):
        self.name = name
        self.w = None
        self.r = []


class Op:
    __slots__ = ("eng", "fn", "deps", "needs_inc", "ev", "is_dma", "tag", "idx")


class Prog:
    ENGS = ("pe", "act", "dve", "pool", "sp")

    def __init__(self, nc, stack):
        self.nc = nc
        self.stack = stack
        self.ops = []
        self.eng_obj = {"pe": nc.tensor, "act": nc.scalar, "dve": nc.vector,
                        "pool": nc.gpsimd, "sp": nc.sync}
        self.nsem = 0
        self.bufs = []
        self.last = {}
        self.dmas = []
        self.emitted = 0
        self.eng_sem = {}
        self.eng_cnt = {}
        self.tag_sem = {}
        self.tag_cnt = {}
        self.waited = {e: {} for e in self.ENGS}
        self.n_ops = 0

    def buf(self, name=""):
        b = Buf(name)
        self.bufs.append(b)
        return b

    def drop(self, bufs):
        s = set(id(b) for b in bufs)
        self.bufs = [b for b in self.bufs if id(b) not in s]

    def op(self, eng, fn, reads=(), writes=(), dma_tag=None, extra_deps=()):
        o = Op()
        o.eng = eng
        o.fn = fn
        o.needs_inc = False
        o.ev = None
        o.is_dma = dma_tag is not None
        o.tag = dma_tag
        o.idx = self.n_ops
        self.n_ops += 1
        deps = list(extra_deps)
        for b in reads:
            if b.w is not None:
                deps.append(b.w)
        for b in writes:
            if b.w is not None:
                deps.append(b.w)
            deps.extend(b.r)
        seen = set()
        dd = []
        for d in deps:
            if eng == "pe" and d.eng == "pe" and not d.is_dma:
                continue
            if d.idx not in seen and d is not o:
                seen.add(d.idx)
                dd.append(d)
        o.deps = dd
        for d in dd:
            d.needs_inc = True
        for b in reads:
            if not o.is_dma:
                b.r = [r for r in b.r if r.is_dma or r.eng != eng]
            b.r.append(o)
        for b in writes:
            b.w = o
            b.r = []
        self.ops.append(o)
        self.last[eng] = o
        if o.is_dma:
            self.dmas.append(o)
        return o

    def barrier(self):
        deps = list(self.last.values()) + list(self.dmas)
        self.dmas = []
        for eng in self.ENGS:
            self.op(eng, lambda e: e.nop(), extra_deps=deps)

    def new_sem(self, name):
        self.nsem += 1
        return self.stack.enter_context(self.nc.semaphore(f"{name}_{self.nsem}"))

    def flush(self, final_wait_ops=()):
        LIM = 30000
        for o in final_wait_ops:
            o.needs_inc = True
        for b in self.bufs:
            if b.w is not None:
                b.w.needs_inc = True
            for r in b.r:
                r.needs_inc = True
        for o in self.last.values():
            o.needs_inc = True
        for o in self.ops:
            e = self.eng_obj[o.eng]
            need = {}
            for d in o.deps:
                sem, val = d.ev
                k = id(sem)
                if k not in need or need[k][1] < val:
                    need[k] = (sem, val)
            w = self.waited[o.eng]
            for k, (sem, val) in need.items():
                cur = w.get(k)
                if cur is None or cur < val:
                    e.wait_ge(sem, val)
                    w[k] = val
            ins = o.fn(e)
            if o.is_dma:
                t = (o.eng, o.tag)
                if t not in self.tag_sem or self.tag_cnt[t] + 16 > LIM:
                    self.tag_sem[t] = self.new_sem("d" + o.eng + str(o.tag))
                    self.tag_cnt[t] = 0
                self.tag_cnt[t] += 16
                ins.then_inc(self.tag_sem[t], 16)
                o.ev = (self.tag_sem[t], self.tag_cnt[t])
            elif o.needs_inc:
                if o.eng not in self.eng_sem or self.eng_cnt[o.eng] + 1 > LIM:
                    self.eng_sem[o.eng] = self.new_sem("e" + o.eng)
                    self.eng_cnt[o.eng] = 0
                self.eng_cnt[o.eng] += 1
                ins.then_inc(self.eng_sem[o.eng], 1)
                o.ev = (self.eng_sem[o.eng], self.eng_cnt[o.eng])
            o.fn = None
        self.ops = []
        fw = {}
        for o in final_wait_ops:
            sem, val = o.ev
            if id(sem) not in fw or fw[id(sem)][1] < val:
                fw[id(sem)] = (sem, val)
        for sem, val in fw.values():
            self.nc.sync.wait_ge(sem, val)


class _Stop(Exception):
    pass


def V(base, extra, dims):
    return bass.AP(base.tensor, base.offset + extra, [list(base.ap[0])] + [list(d) for d in dims])


def build(nl=NL, dbg=(), stop_after=None, ngroups=8):
    nc = bass.Bass("TRN2", target_bir_lowering=False)

    def din(name, shape):
        return nc.dram_tensor(name, shape, F32, kind="ExternalInput").ap()

    xT_d = din("xT", [1024, 2048])
    cT_d = din("cT", [128, 8])
    wada_d = din("w_ada", [NL, 1024, 6144])
    bada_d = din("b_adaT", [NL, 128, 48])
    gains_d = din("gains", [NL, 128, 32])
    sinks_d = din("sinks", [NL, 128, 8])
    win_d = din("w_in_c", [NL, 11, 128, 1024])
    poolw_d = din("pool_w", [NL, 4, 128, 128])
    wout_d = din("w_out_c", [NL, 8, 128, 1024])
    wq_d = din("wq_c", [NL, 16, 128, 1024])
    skT_d = din("skT", [NL, 2, 128, 128])
    uT_d = din("uT", [NL, 128, 128, 1024])
    v_d = din("peer_v", [NL, 16384, 1024])
    ident_d = din("ident", [128, 128])
    biasT_d = din("biasT", [2, 128, 1024])
    fix_d = din("poolfix", [128, 64])
    oT_d = nc.dram_tensor("oT", [1024, 2048], F32, kind="ExternalOutput").ap()
    cu_d = nc.dram_tensor("cache_u", [128, 128, 1024], BF16, kind="Internal").ap()
    cv_d = nc.dram_tensor("cache_v", [128, 128, 1024], BF16, kind="Internal").ap()
    dbg_d = {}
    for name, shape in dbg:
        dbg_d[name] = nc.dram_tensor("dbg_" + name, list(shape), F32, kind="ExternalOutput").ap()
    dbg_ops = []

    with ExitStack() as st:
        P = Prog(nc, st)

        uniq = [0]

        def sb(stack, name, shape, dt=F32):
            uniq[0] += 1
            return stack.enter_context(nc.sbuf_tensor(f"s{uniq[0]}_{name}", list(shape), dt))

        xT = sb(st, "xT", [128, 8, 2048])
        identf = sb(st, "identf", [128, 128])
        identb = sb(st, "identb", [128, 128], BF16)
        onesb = sb(st, "onesb", [128, 128], BF16)
        bdb = sb(st, "bdb", [128, 128], BF16)
        biasT = sb(st, "biasT", [128, 2, 1024])
        fix = sb(st, "fix", [128, 4, 16])
        cT = sb(st, "cT", [128, 8])
        condb = sb(st, "condb", [128, 8], BF16)
        modT = sb(st, "modT", [128, 48])
        badaT = sb(st, "badaT", [128, 48])
        gains = sb(st, "gains", [128, 32])
        esink = sb(st, "esink", [128, 8])
        a1 = sb(st, "a1", [128, 8])
        a2 = sb(st, "a2", [128, 8])
        qg8 = sb(st, "qg8", [128, 1])
        ps = st.enter_context(nc.psum_tensor("ps", [128, 8, 512], F32))

        BX = [[P.buf(f"x{k}_{g}") for g in range(8)] for k in range(8)]
        PB = [P.buf(f"psb{b}") for b in range(8)]
        B_const = P.buf("const")
        B_mod = P.buf("mod")
        B_small = P.buf("small")
        B_dbg = P.buf("dbg")

        def bx(k, t0, t1):
            return [BX[k][g] for g in range(t0 // 256, (t1 + 255) // 256)]

        def dump(name, src_ap, reads):
            if name in dbg_d:
                dst = dbg_d[name]
                if len(src_ap.shape) == 3 and src_ap.shape[1] * src_ap.shape[2] > 4096:
                    for k in range(src_ap.shape[1]):
                        dbg_ops.append(P.op("pool", lambda e, k=k: e.dma_start(out=dst[:, k, :], in_=src_ap[:, k, :]), reads=reads, writes=[B_dbg], dma_tag="dbg"))
                else:
                    dbg_ops.append(P.op("pool", lambda e: e.dma_start(out=dst, in_=src_ap), reads=reads, writes=[B_dbg], dma_tag="dbg"))

        for k in range(8):
            P.op("sp", lambda e, k=k: e.dma_start(out=xT[:, k, :], in_=xT_d[k * 128:(k + 1) * 128, :]),
                 writes=BX[k], dma_tag=f"x{k}")
        P.op("sp", lambda e: e.dma_start(out=identf[:], in_=ident_d), writes=[B_const], dma_tag="cc")
        P.op("pool", lambda e: e.dma_start(out=identb[:], in_=ident_d), writes=[B_const], dma_tag="cc")
        P.op("sp", lambda e: e.dma_start(out=biasT[:], in_=biasT_d.rearrange("k p n -> p k n")), writes=[B_const], dma_tag="cc")
        P.op("sp", lambda e: e.dma_start(out=fix[:].rearrange("p g n -> p (g n)"), in_=fix_d), writes=[B_const], dma_tag="cc")
        P.op("sp", lambda e: e.dma_start(out=cT[:], in_=cT_d), writes=[B_const], dma_tag="cc")
        P.op("dve", lambda e: e.memset(onesb[:], 1.0), writes=[B_const])
        P.op("dve", lambda e: e.memset(bdb[:], 0.0), writes=[B_const])
        P.op("dve", lambda e: e.memset(bdb[0:64, 0:64], 1.0), writes=[B_const])
        P.op("dve", lambda e: e.memset(bdb[64:128, 64:128], 1.0), writes=[B_const])
        P.op("act", lambda e: e.activation(out=condb[:], in_=cT[:], func=AF.Silu), reads=[B_const], writes=[B_const])

        def norm_mod(stk, xlo, n, a_t, sh_col0, out_ap_fn, out_bufs_fn, bank, tmp_tag):
            sqb = [sb(stk, f"sqb{tmp_tag}{i}", [128, n], BF16) for i in range(2)]
            rs = sb(stk, f"rs{tmp_tag}", [128, n])
            tmp = [sb(stk, f"nt{tmp_tag}{i}", [128, n]) for i in range(2)]
            Bsq = [P.buf() for _ in range(2)]
            Brs = P.buf()
            Btmp = [P.buf() for _ in range(2)]

            def run(t0):
                for k in range(8):
                    P.op("act", lambda e, k=k: e.activation(out=sqb[k % 2][:], in_=xT[:, k, t0:t0 + n], func=AF.Square),
                         reads=bx(k, t0, t0 + n), writes=[Bsq[k % 2]])
                    P.op("pe", lambda e, k=k: e.matmul(ps[:, bank, 0:n], lhsT=onesb[:], rhs=sqb[k % 2][:], start=(k == 0), stop=(k == 7)),
                         reads=[Bsq[k % 2], B_const], writes=[PB[bank]])
                P.op("act", lambda e: e.activation(out=rs[:], in_=ps[:, bank, 0:n], func=AF.Ln, scale=1.0 / D, bias=EPS),
                     reads=[PB[bank]], writes=[Brs])
                P.op("act", lambda e: e.activation(out=rs[:], in_=rs[:], func=AF.Exp, scale=-0.5), reads=[Brs], writes=[Brs])
                for k in range(8):
                    P.op("dve", lambda e, k=k: e.scalar_tensor_tensor(out=tmp[k % 2][:], in0=xT[:, k, t0:t0 + n], scalar=a_t[:, k:k + 1],
                                                                      in1=rs[:], op0=ALU.mult, op1=ALU.mult),
                         reads=bx(k, t0, t0 + n) + [Brs, B_mod], writes=[Btmp[k % 2]])
                    P.op("act", lambda e, k=k: e.activation(out=out_ap_fn(k, t0), in_=tmp[k % 2][:], func=AF.Identity,
                                                            bias=modT[:, sh_col0 + k:sh_col0 + k + 1], scale=1.0),
                         reads=[Btmp[k % 2], B_mod], writes=out_bufs_fn(k, t0))
            return run

        stopped = [False]

        def chk(name, l):
            if stop_after == (name, l):
                P.barrier()
                P.flush()
                raise _Stop()

        try:
          for l in range(nl):
              with ExitStack() as sa:
                  wada = [sb(sa, f"wada{i}", [128, 8, 1024], BF16) for i in range(2)]
                  Bw = [P.buf() for _ in range(2)]
                  P.op("sp", lambda e, l=l: e.dma_start(out=badaT[:], in_=bada_d[l]), writes=[B_mod], dma_tag="cm")
                  P.op("sp", lambda e, l=l: e.dma_start(out=gains[:], in_=gains_d[l]), writes=[B_mod], dma_tag="cm")
                  P.op("sp", lambda e, l=l: e.dma_start(out=esink[:], in_=sinks_d[l]), writes=[B_mod], dma_tag="cm")
                  for m in range(6):
                      P.op("pool", lambda e, m=m, l=l: e.dma_start(
                          out=wada[m % 2][:], in_=wada_d[l][:, m * 1024:(m + 1) * 1024].rearrange("(k p) n -> p k n", p=128)),
                          writes=[Bw[m % 2]], dma_tag=f"wa{m % 2}")

                      def mm_ada(e, m=m):
                          ins = None
                          for j in range(8):
                              for k in range(8):
                                  ins = e.matmul(ps[:, 0, m * 8 + j:m * 8 + j + 1], lhsT=wada[m % 2][:, k, j * 128:(j + 1) * 128],
                                                 rhs=condb[:, k:k + 1], start=(k == 0), stop=(k == 7))
                          return ins
                      P.op("pe", mm_ada, reads=[Bw[m % 2], B_const], writes=[PB[0]])
                  P.op("dve", lambda e: e.tensor_tensor(out=modT[:], in0=ps[:, 0, 0:48], in1=badaT[:], op=ALU.add),
                       reads=[PB[0], B_mod], writes=[B_mod])
                  P.op("dve", lambda e: e.scalar_tensor_tensor(out=a1[:], in0=modT[:, 8:16], scalar=1.0, in1=gains[:, 0:8], op0=ALU.add, op1=ALU.mult),
                       reads=[B_mod], writes=[B_mod])
                  P.op("dve", lambda e: e.scalar_tensor_tensor(out=a2[:], in0=modT[:, 32:40], scalar=1.0, in1=gains[:, 8:16], op0=ALU.add, op1=ALU.mult),
                       reads=[B_mod], writes=[B_mod])
                  P.op("act", lambda e: e.activation(out=esink[:], in_=esink[:], func=AF.Exp), reads=[B_mod], writes=[B_mod])
                  P.op("dve", lambda e: e.tensor_scalar(out=qg8[:], in0=gains[:, 28:29], scalar1=0.125, scalar2=None, op0=ALU.mult),
                       reads=[B_mod], writes=[B_mod])
                  dump(f"mod{l}", modT[:], [B_mod])
                  P.barrier()
                  P.flush()
                  P.drop(Bw)
              if stop_after == ("ada", l):
                  break

              with ExitStack() as s1:
                  try:
                      hT = sb(s1, "hT", [128, 8, 2048], BF16)
                      BH = [[P.buf() for _ in range(4)] for _ in range(8)]
                      ypr = sb(s1, "ypr", [128, 4, 2048], BF16)
                      BYP = [[P.buf() for _ in range(4)] for _ in range(4)]
                      pT = sb(s1, "pT", [128, 2048])
                      tA = sb(s1, "tA", [128, 2048])
                      tB = sb(s1, "tB", [128, 2048])
                      pooled = sb(s1, "pooled", [128, 2048], BF16)
                      B_pT, B_tA, B_tB, B_pooled = P.buf(), P.buf(), P.buf(), P.buf()
                      qnT = sb(s1, "qnT", [128, 4, 2048], BF16)
                      knT = sb(s1, "knT", [128, 2, 2048], BF16)
                      BQ = [[P.buf() for _ in range(4)] for _ in range(4)]
                      BK = [[P.buf() for _ in range(4)] for _ in range(2)]
                      v_sb = sb(s1, "v_sb", [128, 16, 2, 65], BF16)
                      BV = [P.buf() for _ in range(16)]
                      wch = [sb(s1, f"wch{i}", [128, 8, 128], BF16) for i in range(2)]
                      BW = [P.buf() for _ in range(2)]
                      poolw = sb(s1, "poolw", [128, 4, 128], BF16)
                      B_pw = P.buf()
                      local_bufs = [b for row in BH for b in row] + [b for row in BYP for b in row] + [B_pT, B_tA, B_tB, B_pooled] + \
                          [b for row in BQ for b in row] + [b for row in BK for b in row] + BV + BW + [B_pw]
                      wslot = [0]

                      def load_w(src_ap):
                          i = wslot[0] % 2
                          wslot[0] += 1
                          P.op("pool", lambda e: e.dma_start(out=wch[i][:].rearrange("p k n -> p (k n)"), in_=src_ap), writes=[BW[i]], dma_tag=f"w{i}")
                          return i

                      P.op("pool", lambda e, l=l: e.dma_start(out=poolw[:], in_=poolw_d[l].rearrange("g c d -> c g d")), writes=[B_pw], dma_tag="pw")
                      P.op("dve", lambda e: e.memset(v_sb[:, :, :, 64:65], 1.0), writes=BV)

                      nm = norm_mod(s1, 0, 512, a1, 0, lambda k, t0: hT[:, k, t0:t0 + 512], lambda k, t0: [BH[k][t0 // 512]], 1, "a")
                      for tg in range(4):
                          nm(tg * 512)

                      dump(f"h{l}", hT[:], [b for row in BH for b in row])
                      chk("norm", l)
                      qraw = sb(s1, "qraw", [128, 512])
                      qsq = sb(s1, "qsq", [128, 512], BF16)
                      qrs = sb(s1, "qrs", [128, 512])
                      B_qraw, B_qsq, B_qrs = P.buf(), P.buf(), P.buf()
                      local_bufs += [B_qraw, B_qsq, B_qrs]
                      pbank = [0]
                      for oc in range(4, 10):
                          wi = load_w(win_d[l, oc])
                          for tg in range(4):
                              bk = 2 + (pbank[0] % 2)
                              pbank[0] += 1

                              def mmq(e, wi=wi, tg=tg, bk=bk):
                                  ins = None
                                  for k in range(8):
                                      ins = e.matmul(ps[:, bk, :], lhsT=wch[wi][:, k, :], rhs=hT[:, k, tg * 512:(tg + 1) * 512], start=(k == 0), stop=(k == 7))
                                  return ins
                              P.op("pe", mmq, reads=[BW[wi]] + [BH[k][tg] for k in range(8)], writes=[PB[bk]])
                              P.op("act", lambda e, bk=bk: e.activation(out=qraw[:], in_=ps[:, bk, :], func=AF.Copy), reads=[PB[bk]], writes=[B_qraw])
                              P.op("act", lambda e: e.activation(out=qsq[:], in_=qraw[:], func=AF.Square), reads=[B_qraw], writes=[B_qsq])
                              P.op("pe", lambda e: e.matmul(ps[:, 1, :], lhsT=bdb[:], rhs=qsq[:], start=True, stop=True), reads=[B_qsq, B_const], writes=[PB[1]])
                              P.op("act", lambda e: e.activation(out=qrs[:], in_=ps[:, 1, :], func=AF.Ln, scale=1.0 / 64, bias=EPS), reads=[PB[1]], writes=[B_qrs])
                              P.op("act", lambda e: e.activation(out=qrs[:], in_=qrs[:], func=AF.Exp, scale=-0.5), reads=[B_qrs], writes=[B_qrs])
                              if oc < 8:
                                  dst, dbuf, gcol = qnT[:, oc - 4, tg * 512:(tg + 1) * 512], BQ[oc - 4][tg], qg8[:, 0:1]
                              else:
                                  dst, dbuf, gcol = knT[:, oc - 8, tg * 512:(tg + 1) * 512], BK[oc - 8][tg], gains[:, 29:30]
                              P.op("dve", lambda e, dst=dst, gcol=gcol: e.scalar_tensor_tensor(out=dst, in0=qraw[:], scalar=gcol, in1=qrs[:], op0=ALU.mult, op1=ALU.mult),
                                   reads=[B_qraw, B_qrs, B_mod], writes=[dbuf])

                      dump(f"qn{l}", qnT[:], [b for row in BQ for b in row])
                      dump(f"kn{l}", knT[:], [b for row in BK for b in row])
                      chk("qk", l)
                      wi = load_w(win_d[l, 10])
                      for tt in range(16):
                          bk = 2 + (pbank[0] % 2)
                          pbank[0] += 1

                          def mmv(e, wi=wi, tt=tt, bk=bk):
                              ins = None
                              for k in range(8):
                                  ins = e.matmul(ps[:, bk, 0:128], lhsT=hT[:, k, tt * 128:(tt + 1) * 128], rhs=wch[wi][:, k, :], start=(k == 0), stop=(k == 7))
                              return ins
                          P.op("pe", mmv, reads=[BW[wi]] + [BH[k][tt // 4] for k in range(8)], writes=[PB[bk]])
                          P.op("act", lambda e, tt=tt, bk=bk: e.activation(out=v_sb[:, tt, :, 0:64], in_=ps[:, bk, 0:128].rearrange("p (a b) -> p a b", a=2), func=AF.Copy),
                               reads=[PB[bk]], writes=[BV[tt]])

                      dump(f"v{l}", v_sb[:], BV)
                      chk("v", l)
                      for g in range(4):
                          w = (2, 4, 8, 16)[g]
                          wi = load_w(win_d[l, g])
                          for tg in range(4):
                              bk = 2 + (pbank[0] % 2)
                              pbank[0] += 1

                              def mmp(e, wi=wi, tg=tg, bk=bk):
                                  ins = None
                                  for k in range(8):
                                      ins = e.matmul(ps[:, bk, :], lhsT=wch[wi][:, k, :], rhs=hT[:, k, tg * 512:(tg + 1) * 512], start=(k == 0), stop=(k == 7))
                                  return ins
                              P.op("pe", mmp, reads=[BW[wi]] + [BH[k][tg] for k in range(8)], writes=[PB[bk]])
                              P.op("act", lambda e, tg=tg, bk=bk: e.activation(out=pT[:, tg * 512:(tg + 1) * 512], in_=ps[:, bk, :], func=AF.Copy), reads=[PB[bk]], writes=[B_pT])
                          src, Bsrc = pT, B_pT
                          bufs2 = [(tA, B_tA), (tB, B_tB)]
                          sh = 1
                          step = 0
                          while sh < w:
                              dst, Bdst = bufs2[step % 2]
                              eng = "dve" if step % 2 == 0 else "pool"
                              P.op(eng, lambda e, dst=dst, src=src, sh=sh: e.tensor_tensor(out=dst[:, sh:], in0=src[:, sh:], in1=src[:, :S - sh], op=ALU.add),
                                   reads=[Bsrc], writes=[Bdst])
                              P.op(eng, lambda e, dst=dst, src=src, sh=sh: e.tensor_copy(out=dst[:, 0:sh], in_=src[:, 0:sh]), reads=[Bsrc], writes=[Bdst])
                              src, Bsrc = dst, Bdst
                              sh *= 2
                              step += 1
                          P.op("dve", lambda e, src=src, g=g: e.tensor_tensor(out=src[:, 0:16], in0=src[:, 0:16], in1=fix[:, g, :], op=ALU.mult),
                               reads=[Bsrc, B_const], writes=[Bsrc])
                          P.op("dve", lambda e, src=src, w=w: e.scalar_tensor_tensor(out=pooled[:], in0=src[:], scalar=1.0 / w, in1=pT[:], op0=ALU.mult, op1=ALU.subtract),
                               reads=[Bsrc, B_pT], writes=[B_pooled])
                          for tg in range(4):
                              bk = 2 + (pbank[0] % 2)
                              pbank[0] += 1
                              P.op("pe", lambda e, g=g, tg=tg, bk=bk: e.matmul(ps[:, bk, :], lhsT=poolw[:, g, :], rhs=pooled[:, tg * 512:(tg + 1) * 512], start=True, stop=True),
                                   reads=[B_pw, B_pooled], writes=[PB[bk]])
                              P.op("act", lambda e, g=g, tg=tg, bk=bk: e.activation(out=ypr[:, g, tg * 512:(tg + 1) * 512], in_=ps[:, bk, :], func=AF.Identity,
                                                                                 scale=gains[:, 24 + g:25 + g]),
                                   reads=[PB[bk], B_mod], writes=[BYP[g][tg]])

                      dump(f"ypr{l}", ypr[:], [b for row in BYP for b in row])
                      chk("pool", l)
                      prs, B_prs = qrs, B_qrs
                      for tg in range(4):
                          for g in range(4):
                              P.op("act", lambda e, g=g, tg=tg: e.activation(out=qsq[:], in_=ypr[:, g, tg * 512:(tg + 1) * 512], func=AF.Square),
                                   reads=[BYP[g][tg]], writes=[B_qsq])
                              P.op("pe", lambda e, g=g: e.matmul(ps[:, 1, :], lhsT=onesb[:], rhs=qsq[:], start=(g == 0), stop=(g == 3)),
                                   reads=[B_qsq, B_const], writes=[PB[1]])
                          P.op("act", lambda e: e.activation(out=prs[:], in_=ps[:, 1, :], func=AF.Ln, scale=1.0 / 512, bias=EPS), reads=[PB[1]], writes=[B_prs])
                          P.op("act", lambda e: e.activation(out=prs[:], in_=prs[:], func=AF.Exp, scale=-0.5), reads=[B_prs], writes=[B_prs])
                          for g in range(4):
                              P.op("dve", lambda e, g=g, tg=tg: e.scalar_tensor_tensor(out=hT[:, g, tg * 512:(tg + 1) * 512], in0=ypr[:, g, tg * 512:(tg + 1) * 512],
                                                                                   scalar=gains[:, 16 + g:17 + g], in1=prs[:], op0=ALU.mult, op1=ALU.mult),
                                   reads=[BYP[g][tg], B_prs, B_mod], writes=[BH[g][tg]])

                      chk("poolnorm", l)
                      ssb = sb(s1, "ssb", [128, 1024])
                      PT = sb(s1, "PT", [128, 1024], BF16)
                      B_ssb, B_PT = P.buf(), P.buf()
                      den = sb(s1, "den", [128, 8])
                      yatt = sb(s1, "yatt", [128, 8, 64])
                      ssq = sb(s1, "ssq", [128, 2])
                      yab = sb(s1, "yab", [128, 512], BF16)
                      B_den, B_yatt, B_ssq, B_yab = P.buf(), P.buf(), P.buf(), P.buf()
                      yjunk, B_yjunk = ssb, B_ssb
                      local_bufs += [B_ssb, B_PT, B_den, B_yatt, B_ssq, B_yab]
                      for n in range(16):
                          js = [1] if n == 0 else [0, 1]
                          for kh in range(2):
                              def mms(e, n=n, kh=kh):
                                  ins = None
                                  for j in range(2):
                                      kb = max(n - 1 + j, 0)
                                      for g in range(4):
                                          h = kh * 4 + g
                                          r = h % 2
                                          lo = r * 64
                                          col = (j * 2 + g // 2) * 128
                                          ins = e.matmul(ps[:, 4 + r, col:col + 128], lhsT=knT[lo:lo + 64, kh, kb * 128:(kb + 1) * 128],
                                                         rhs=qnT[lo:lo + 64, h // 2, n * 128:(n + 1) * 128], start=True, stop=True)
                                  return ins
                              rd = [BK[kh][(n - 1) // 4 if n > 0 else 0], BK[kh][n // 4], BQ[kh * 2][n // 4], BQ[kh * 2 + 1][n // 4]]
                              P.op("pe", mms, reads=rd, writes=[PB[4], PB[5]])
                              P.op("dve", lambda e, kh=kh: e.tensor_tensor(out=ssb[:], in0=V(ps[:, 4, :], 0, [[1, 1024]]),
                                                                          in1=biasT[:, kh, :], op=ALU.add),
                                   reads=[PB[4], PB[5], B_const], writes=[B_ssb])
                              P.op("act", lambda e: e.activation(out=PT[:], in_=ssb[:], func=AF.Exp), reads=[B_ssb], writes=[B_PT])

                              def mmo(e, n=n, kh=kh, js=js):
                                  ins = None
                                  for g in range(4):
                                      for j in js:
                                          kb = n - 1 + j
                                          col = (g % 2) * 512 + (j * 2 + g // 2) * 128
                                          ins = e.matmul(ps[:, 6 + kh, g * 65:(g + 1) * 65], lhsT=PT[:, col:col + 128],
                                                         rhs=v_sb[:, kb, kh, :], start=(j == js[0]), stop=(j == 1))
                                  return ins
                              P.op("pe", mmo, reads=[B_PT, BV[max(n - 1, 0)], BV[n]], writes=[PB[6 + kh]])
                          for kh in range(2):
                              P.op("dve", lambda e, kh=kh: e.tensor_tensor(out=den[:, kh * 4:(kh + 1) * 4], in0=V(ps[:, 6 + kh, :], 64, [[65, 4]]),
                                                                          in1=esink[:, kh * 4:(kh + 1) * 4], op=ALU.add),
                                   reads=[PB[6 + kh], B_mod], writes=[B_den])
                          P.op("dve", lambda e: e.reciprocal(out=den[:], in_=den[:]), reads=[B_den], writes=[B_den])
                          for kh in range(2):
                              P.op("dve", lambda e, kh=kh: e.tensor_tensor(out=yatt[:, kh * 4:(kh + 1) * 4, :], in0=V(ps[:, 6 + kh, :], 0, [[65, 4], [1, 64]]),
                                                                          in1=V(den[:], kh * 4, [[1, 4], [0, 64]]), op=ALU.mult),
                                   reads=[PB[6 + kh], B_den], writes=[B_yatt])
                          P.op("act", lambda e: e.activation(out=yjunk[:, 0:512], in_=yatt[:].rearrange("p a b -> p (a b)"), func=AF.Square, accum_out=ssq[:, 0:1]),
                               reads=[B_yatt], writes=[B_ssq, B_yjunk])
                          P.op("act", lambda e: e.activation(out=ssq[:, 1:2], in_=ssq[:, 0:1], func=AF.Ln, scale=1.0 / 512, bias=EPS), reads=[B_ssq], writes=[B_ssq])
                          P.op("act", lambda e: e.activation(out=ssq[:, 1:2], in_=ssq[:, 1:2], func=AF.Exp, scale=-0.5), reads=[B_ssq], writes=[B_ssq])
                          P.op("dve", lambda e: e.tensor_scalar(out=yab[:], in0=yatt[:].rearrange("p a b -> p (a b)"), scalar1=ssq[:, 1:2], scalar2=None, op0=ALU.mult),
                               reads=[B_yatt, B_ssq], writes=[B_yab])
                          psb = ps[:, 1, :].bitcast(BF16)

                          def trn(e):
                              ins = None
                              for c in range(4):
                                  ins = e.transpose(psb[:, c * 128:(c + 1) * 128], yab[:, c * 128:(c + 1) * 128], identb[:])
                              return ins
                          P.op("pe", trn, reads=[B_yab, B_const], writes=[PB[1]])
                          for c in range(4):
                              P.op("act", lambda e, c=c, n=n: e.activation(out=hT[:, 4 + c, n * 128:(n + 1) * 128], in_=psb[:, c * 128:(c + 1) * 128], func=AF.Identity,
                                                                        scale=gains[:, 20 + c:21 + c]),
                                   reads=[PB[1], B_mod], writes=[BH[4 + c][n // 4]])
                      dump(f"y{l}", hT[:], [b for row in BH for b in row])

                      chk("attn", l)
                      for oc in range(8):
                          wi = load_w(wout_d[l, oc])
                          for tg in range(4):
                              bk = 2 + (pbank[0] % 2)
                              pbank[0] += 1

                              def mmw(e, wi=wi, tg=tg, bk=bk):
                                  ins = None
                                  for k in range(8):
                                      ins = e.matmul(ps[:, bk, :], lhsT=wch[wi][:, k, :], rhs=hT[:, k, tg * 512:(tg + 1) * 512], start=(k == 0), stop=(k == 7))
                                  return ins
                              P.op("pe", mmw, reads=[BW[wi]] + [BH[k][tg] for k in range(8)], writes=[PB[bk]])
                              P.op("dve", lambda e, oc=oc, tg=tg, bk=bk: e.scalar_tensor_tensor(out=xT[:, oc, tg * 512:(tg + 1) * 512], in0=ps[:, bk, :],
                                                                                              scalar=modT[:, 16 + oc:17 + oc], in1=xT[:, oc, tg * 512:(tg + 1) * 512],
                                                                                              op0=ALU.mult, op1=ALU.add),
                                   reads=[PB[bk], B_mod] + bx(oc, tg * 512, (tg + 1) * 512), writes=bx(oc, tg * 512, (tg + 1) * 512))
                      dump(f"x1_{l}", xT[:], [b for row in BX for b in row])
                      P.barrier()
                      P.flush()
                      P.drop(local_bufs)
                  except _Stop:
                      stopped[0] = True
              if stopped[0]:
                  break
              if stop_after == ("s1", l):
                  break

              with ExitStack() as s2:
                  try:
                      GT = sb(s2, "GT", [128, 256, 128], BF16)
                      B_GT = P.buf()
                      h2g = sb(s2, "h2g", [128, 8, 256], BF16)
                      B_h2 = [P.buf() for _ in range(8)]
                      NSL = 7
                      reg = sb(s2, "reg", [128, NSL * 2048], BF16)
                      qTg = reg[:, 0:4096].rearrange("p (a t) -> p a t", a=16)
                      B_qT = [P.buf() for _ in range(16)]

                      def rv(s_, i):
                          b0 = 4096 + s_ * 3584 + i * 512
                          return reg[:, b0:b0 + 512].rearrange("p (t n) -> p t n", t=4)
                      qrep = [[rv(s_, 0), rv(s_, 1)] for s_ in range(2)]
                      EQ = [rv(s_, 2) for s_ in range(2)]
                      Lm = [rv(s_, 3) for s_ in range(2)]
                      E2 = [rv(s_, 4) for s_ in range(2)]
                      Rm = [rv(s_, 5) for s_ in range(2)]
                      Mk = [rv(s_, 6) for s_ in range(2)]
                      B_qrep = [[P.buf(), P.buf()] for _ in range(2)]
                      B_EQ = [P.buf() for _ in range(2)]
                      B_L = [P.buf() for _ in range(2)]
                      B_E2 = [P.buf() for _ in range(2)]
                      B_R = [P.buf() for _ in range(2)]
                      B_M = [P.buf() for _ in range(2)]
                      ub = [reg[:, i * 2048:i * 2048 + 1024].rearrange("p (k n) -> p k n", k=8) for i in range(NSL)]
                      vb = [reg[:, i * 2048 + 1024:(i + 1) * 2048] for i in range(NSL)]
                      B_ub = [P.buf() for _ in range(NSL)]
                      B_vb = [P.buf() for _ in range(NSL)]
                      fdum = sb(s2, "fdum", [128, 2])
                      B_fd = P.buf()
                      region_bufs = B_qT + [b for r_ in B_qrep for b in r_] + B_EQ + B_L + B_E2 + B_R + B_M + B_ub + B_vb

                      def fence():
                          P.op("pool", lambda e: e.memset(fdum[:], 0.0), writes=region_bufs + [B_fd])

                      s_sb = sb(s2, "s_sb", [128, 16, 128])
                      wk = sb(s2, "wk", [128, 16, 128])
                      B_s, B_wk = P.buf(), P.buf()
                      top = sb(s2, "top", [128, 16, 16])
                      c16 = sb(s2, "c16", [128, 8, 16])
                      e16 = sb(s2, "e16", [128, 8, 16])
                      Zt = sb(s2, "Zt", [128, 8])
                      tme = sb(s2, "tme", [128, 8])
                      tok3 = sb(s2, "tok3", [128, 3, 128])
                      B_top, B_c16, B_e16, B_Z, B_tok3 = P.buf(), P.buf(), P.buf(), P.buf(), P.buf()
                      B_tophp = [P.buf() for _ in range(16)]
                      B_wkhp = [P.buf() for _ in range(16)]
                      B_c16h = [P.buf() for _ in range(8)]
                      slotT = sb(s2, "slotT", [128, 3, 256])
                      B_slot = P.buf()
                      skb = sb(s2, "skb", [128, 2, 128], BF16)
                      B_sk = P.buf()
                      gl = [sb(s2, f"gl{i}", [128, 256]) for i in range(2)]
                      actT = [sb(s2, f"actT{i}", [128, 256], BF16) for i in range(2)]
                      B_gl = [P.buf() for _ in range(2)]
                      B_act = [P.buf() for _ in range(2)]
                      wqc = [sb(s2, f"wqc{i}", [128, 8, 128], BF16) for i in range(2)]
                      B_wq = [P.buf() for _ in range(2)]
                      B_cu = [P.buf() for _ in range(128)]
                      B_cv = [P.buf() for _ in range(128)]
                      local_bufs = [B_GT, B_s, B_wk, B_top, B_c16, B_e16, B_Z, B_tok3, B_slot, B_sk, B_fd] + B_tophp + B_wkhp + B_c16h + \
                          B_h2 + region_bufs + B_gl + B_act + B_wq + B_cu + B_cv

                      P.op("pool", lambda e, l=l: e.dma_start(out=skb[:], in_=skT_d[l].rearrange("a d n -> d a n")), writes=[B_sk], dma_tag="sk")
                      nm2 = norm_mod(s2, 0, 256, a2, 24, lambda k, t0: h2g[:, k, :], lambda k, t0: [B_h2[k]], 7, "b")
                      cand = s_sb[:].rearrange("p a b -> p (a b)").rearrange("p (h n) -> p h n", h=8)
                      cwk = wk[:].rearrange("p a b -> p (a b)").rearrange("p (h n) -> p h n", h=8)

                      for G in range(ngroups):
                          t0 = G * 256
                          nm2(t0)
                          fence()
                          for hp in range(16):
                              i = hp % 2
                              P.op("pool", lambda e, i=i, hp=hp, l=l: e.dma_start(out=wqc[i][:].rearrange("p k n -> p (k n)"), in_=wq_d[l, hp]),
                                   writes=[B_wq[i]], dma_tag=f"wq{i}")
                              bk = 6 + (hp % 2)

                              def mmq2(e, i=i, bk=bk):
                                  ins = None
                                  for k in range(8):
                                      ins = e.matmul(ps[:, bk, 0:256], lhsT=wqc[i][:, k, :], rhs=h2g[:, k, :], start=(k == 0), stop=(k == 7))
                                  return ins
                              P.op("pe", mmq2, reads=[B_wq[i]] + B_h2, writes=[PB[bk]])
                              P.op("act", lambda e, hp=hp, bk=bk: e.activation(out=qTg[:, hp, :], in_=ps[:, bk, 0:256], func=AF.Copy), reads=[PB[bk]], writes=[B_qT[hp]])
                          for tt in range(2):
                              def mmsc(e, tt=tt):
                                  ins = None
                                  for hp in range(16):
                                      ins = e.matmul(ps[:, hp // 4, (hp % 4) * 128:(hp % 4 + 1) * 128], lhsT=qTg[:, hp, tt * 128:(tt + 1) * 128],
                                                     rhs=skb[:, hp % 2, :], start=True, stop=True)
                                  return ins
                              P.op("pe", mmsc, reads=B_qT + [B_sk], writes=PB[0:4])
                              for b4 in range(4):
                                  if b4 % 2 == 0:
                                      P.op("act", lambda e, b4=b4: e.activation(out=s_sb[:, b4 * 4:(b4 + 1) * 4, :].rearrange("p a b -> p (a b)"), in_=ps[:, b4, :], func=AF.Copy),
                                           reads=[PB[b4]], writes=[B_s])
                                  else:
                                      P.op("dve", lambda e, b4=b4: e.tensor_copy(out=s_sb[:, b4 * 4:(b4 + 1) * 4, :].rearrange("p a b -> p (a b)"), in_=ps[:, b4, :]),
                                           reads=[PB[b4]], writes=[B_s])
                              for hp in range(16):
                                  P.op("dve", lambda e, hp=hp: e.max(out=top[:, hp, 0:8], in_=s_sb[:, hp, :]), reads=[B_s], writes=[B_tophp[hp]])
                              for hp in range(16):
                                  P.op("dve", lambda e, hp=hp: e.match_replace(out=wk[:, hp, :], in_to_replace=top[:, hp, 0:8], in_values=s_sb[:, hp, :], imm_value=-1e30),
                                       reads=[B_s, B_tophp[hp]], writes=[B_wkhp[hp]])
                              for hp in range(16):
                                  P.op("dve", lambda e, hp=hp: e.max(out=top[:, hp, 8:16], in_=wk[:, hp, :]), reads=[B_wkhp[hp]], writes=[B_tophp[hp]])
                              P.op("dve", lambda e: e.tensor_tensor(out=cand.rearrange("p h (a b) -> p h a b", a=16),
                                                                    in0=V(top[:], 0, [[32, 8], [1, 16], [0, 16]]),
                                                                    in1=V(top[:], 16, [[32, 8], [0, 16], [1, 16]]), op=ALU.add),
                                   reads=B_tophp, writes=[B_s])
                              for h in range(8):
                                  P.op("dve", lambda e, h=h: e.max(out=c16[:, h, 0:8], in_=cand[:, h, :]), reads=[B_s], writes=[B_c16h[h]])
                              for h in range(8):
                                  P.op("dve", lambda e, h=h: e.match_replace(out=cwk[:, h, :], in_to_replace=c16[:, h, 0:8], in_values=cand[:, h, :], imm_value=-1e30),
                                       reads=[B_s, B_c16h[h]], writes=[B_wkhp[2 * h], B_wkhp[2 * h + 1]])
                              for h in range(8):
                                  P.op("dve", lambda e, h=h: e.max(out=c16[:, h, 8:16], in_=cwk[:, h, :]), reads=[B_wkhp[2 * h], B_wkhp[2 * h + 1]], writes=[B_c16h[h]])
                              P.op("dve", lambda e: e.tensor_tensor(out=e16[:], in0=c16[:], in1=V(c16[:], 0, [[16, 8], [0, 16]]), op=ALU.subtract),
                                   reads=B_c16h, writes=[B_e16])
                              P.op("act", lambda e: e.activation(out=e16[:], in_=e16[:], func=AF.Exp), reads=[B_e16], writes=[B_e16])
                              P.op("dve", lambda e: e.tensor_reduce(out=Zt[:], in_=e16[:], axis=AX.X, op=ALU.add), reads=[B_e16], writes=[B_Z])
                              P.op("dve", lambda e: e.reciprocal(out=Zt[:], in_=Zt[:]), reads=[B_Z], writes=[B_Z])
                              P.op("dve", lambda e: e.tensor_scalar(out=tme[:], in0=V(c16[:], 15, [[16, 8]]), scalar1=-THR_EPS, scalar2=None, op0=ALU.add),
                                   reads=B_c16h, writes=[B_Z])
                              top1v = V(top[:], 0, [[32, 8], [1, 16]])
                              P.op("dve", lambda e: e.tensor_copy(out=tok3[:, 0, :].rearrange("p (h a) -> p h a", h=8), in_=top1v), reads=B_tophp, writes=[B_tok3])
                              P.op("dve", lambda e: e.tensor_tensor(out=tok3[:, 1, :].rearrange("p (h a) -> p h a", h=8), in0=V(tme[:], 0, [[1, 8], [0, 16]]),
                                                                    in1=top1v, op=ALU.subtract), reads=B_tophp + [B_Z], writes=[B_tok3])
                              P.op("dve", lambda e: e.tensor_tensor(out=e16[:], in0=top1v, in1=V(c16[:], 0, [[16, 8], [0, 16]]), op=ALU.subtract),
                                   reads=B_tophp + B_c16h, writes=[B_e16])
                              P.op("act", lambda e: e.activation(out=e16[:], in_=e16[:], func=AF.Exp), reads=[B_e16], writes=[B_e16])
                              P.op("dve", lambda e: e.tensor_tensor(out=tok3[:, 2, :].rearrange("p (h a) -> p h a", h=8), in0=e16[:], in1=V(Zt[:], 0, [[1, 8], [0, 16]]), op=ALU.mult),
                                   reads=[B_e16, B_Z], writes=[B_tok3])

                              def trs(e):
                                  ins = None
                                  for i in range(3):
                                      ins = e.transpose(ps[:, 4, i * 128:(i + 1) * 128], tok3[:, i, :], identf[:])
                                  return ins
                              P.op("pe", trs, reads=[B_tok3, B_const], writes=[PB[4]])
                              P.op("act", lambda e, tt=tt: e.activation(out=slotT[:, :, tt * 128:(tt + 1) * 128], in_=ps[:, 4, 0:384].rearrange("p (a b) -> p a b", a=3), func=AF.Copy),
                                   reads=[PB[4]], writes=[B_slot])
                          if G == 0:
                              dump(f"slotT{l}", slotT[:], [B_slot])
                          NSB = 64

                          def front(sbi):
                              tl = sbi * 4
                              s_ = sbi % 2
                              for p_ in range(2):
                                  P.op("act", lambda e, p_=p_: e.activation(out=qrep[s_][p_].rearrange("p t (h a) -> p t h a", h=8),
                                                                           in_=V(reg[:], p_ * 256 + tl, [[1, 4], [512, 8], [0, 16]]), func=AF.Copy),
                                       reads=B_qT, writes=[B_qrep[s_][p_]])

                                  def mmrep(e, p_=p_):
                                      ins = None
                                      for i in range(4):
                                          ins = e.matmul(ps[:, 2 * p_ + s_, i * 128:(i + 1) * 128], lhsT=qrep[s_][p_][:, i, :], rhs=skb[:, p_, :], start=True, stop=True)
                                      return ins
                                  P.op("pe", mmrep, reads=[B_qrep[s_][p_], B_sk], writes=[PB[2 * p_ + s_]])
                              ps1 = ps[:, s_, :].rearrange("p (t n) -> p t n", t=4)
                              ps2 = ps[:, 2 + s_, :].rearrange("p (t n) -> p t n", t=4)
                              P.op("dve", lambda e: e.tensor_tensor(out=EQ[s_], in0=ps1, in1=V(slotT[:], 0 * 256 + tl, [[1, 4], [0, 128]]), op=ALU.is_equal),
                                   reads=[PB[s_], B_slot], writes=[B_EQ[s_]])
                              P.op("act", lambda e: e.activation(out=E2[s_], in_=ps2, func=AF.Exp), reads=[PB[2 + s_]], writes=[B_E2[s_]])
                              P.op("dve", lambda e: e.tensor_tensor(out=Mk[s_], in0=ps2, in1=V(slotT[:], 1 * 256 + tl, [[1, 4], [0, 128]]), op=ALU.is_ge),
                                   reads=[PB[2 + s_], B_slot, B_E2[s_]], writes=[B_M[s_]])
                              P.op("dve", lambda e: e.tensor_tensor(out=Lm[s_], in0=EQ[s_], in1=V(slotT[:], 2 * 256 + tl, [[1, 4], [0, 128]]), op=ALU.mult),
                                   reads=[B_EQ[s_], B_slot], writes=[B_L[s_]])
                              P.op("dve", lambda e: e.tensor_tensor(out=Rm[s_], in0=Mk[s_], in1=E2[s_], op=ALU.mult), reads=[B_M[s_], B_E2[s_]], writes=[B_R[s_]])

                          def back(sbi):
                              tl = sbi * 4
                              s_ = sbi % 2

                              def mmg(e):
                                  ins = None
                                  for i in range(4):
                                      ins = e.matmul(ps[:, 4 + s_, i * 128:(i + 1) * 128], lhsT=Rm[s_][:, i, :], rhs=Lm[s_][:, i, :], start=True, stop=True)
                                  return ins
                              P.op("pe", mmg, reads=[B_R[s_], B_L[s_]], writes=[PB[4 + s_]])
                              P.op("act", lambda e: e.activation(out=GT[:, tl:tl + 4, :], in_=ps[:, 4 + s_, :].rearrange("p (t n) -> p t n", t=4), func=AF.Copy),
                                   reads=[PB[4 + s_]], writes=[B_GT])
                          for sbi in range(NSB + 1):
                              if sbi < NSB:
                                  front(sbi)
                              if sbi >= 1:
                                  back(sbi - 1)
                          if G == 0:
                              dump(f"GT{l}", GT[:], [B_GT])
                          fence()
                          def pfront(c):
                              sl = c % NSL
                              if G == 0:
                                  P.op("pool", lambda e: e.dma_start(out=ub[sl].rearrange("p k n -> p (k n)"), in_=uT_d[l, c]), writes=[B_ub[sl]], dma_tag=f"u{sl}")
                                  P.op("pool", lambda e: e.dma_start(out=vb[sl], in_=v_d[l, c * 128:(c + 1) * 128, :]), writes=[B_vb[sl]], dma_tag=f"v{sl}")
                                  P.op("sp", lambda e: e.dma_start(out=cu_d[c], in_=ub[sl].rearrange("p k n -> p (k n)")), reads=[B_ub[sl]], writes=[B_cu[c]], dma_tag=f"wu{sl}")
                                  P.op("sp", lambda e: e.dma_start(out=cv_d[c], in_=vb[sl]), reads=[B_vb[sl]], writes=[B_cv[c]], dma_tag=f"wv{sl}")
                              else:
                                  P.op("sp", lambda e: e.dma_start(out=ub[sl].rearrange("p k n -> p (k n)"), in_=cu_d[c]), reads=[B_cu[c]], writes=[B_ub[sl]], dma_tag=f"cu{sl}")
                                  P.op("sp", lambda e: e.dma_start(out=vb[sl], in_=cv_d[c]), reads=[B_cv[c]], writes=[B_vb[sl]], dma_tag=f"cv{sl}")
                              i2 = c % 2
                              bk = 6 + i2

                              def mma(e):
                                  ins = None
                                  for k in range(8):
                                      ins = e.matmul(ps[:, bk, 0:256], lhsT=ub[sl][:, k, :], rhs=h2g[:, k, :], start=(k == 0), stop=(k == 7))
                                  return ins
                              P.op("pe", mma, reads=[B_ub[sl]] + B_h2, writes=[PB[bk]])
                              P.op("act", lambda e: e.activation(out=gl[i2][:], in_=ps[:, bk, 0:256], func=AF.Gelu), reads=[PB[bk]], writes=[B_gl[i2]])
                              P.op("dve", lambda e: e.tensor_tensor(out=actT[i2][:], in0=gl[i2][:], in1=GT[:, :, c], op=ALU.mult),
                                   reads=[B_gl[i2], B_GT], writes=[B_act[i2]])

                          def pback(c):
                              sl = c % NSL
                              i2 = c % 2

                              def mmo2(e):
                                  ins = None
                                  for fk in range(8):
                                      ins = e.matmul(ps[:, fk // 2, (fk % 2) * 256:(fk % 2 + 1) * 256], lhsT=vb[sl][:, fk * 128:(fk + 1) * 128], rhs=actT[i2][:],
                                                     start=(c == 0 and fk % 2 == 0), stop=(c == 127), skip_group_check=True)
                                  return ins
                              P.op("pe", mmo2, reads=[B_vb[sl], B_act[i2]], writes=PB[0:4])
                          for c in range(129):
                              if c < 128:
                                  pfront(c)
                              if c >= 1:
                                  pback(c - 1)
                          for fk in range(8):
                              P.op("dve", lambda e, fk=fk, t0=t0: e.scalar_tensor_tensor(out=xT[:, fk, t0:t0 + 256], in0=ps[:, fk // 2, (fk % 2) * 256:(fk % 2 + 1) * 256],
                                                                                       scalar=modT[:, 40 + fk:41 + fk], in1=xT[:, fk, t0:t0 + 256], op0=ALU.mult, op1=ALU.add),
                                   reads=[PB[fk // 2], B_mod, BX[fk][G]], writes=[BX[fk][G]])
                          P.flush()
                      dump(f"x2_{l}", xT[:], [b for row in BX for b in row])
                      P.barrier()
                      P.flush()
                      P.drop(local_bufs)
                  except _Stop:
                      stopped[0] = True
              if stopped[0]:
                  break
        except _Stop:
            pass

        fin = list(dbg_ops)
        for k in range(8):
            fin.append(P.op("sp", lambda e, k=k: e.dma_start(out=oT_d[k * 128:(k + 1) * 128, :], in_=xT[:, k, :]), reads=BX[k], dma_tag="o"))
        P.flush(final_wait_ops=fin)
    return nc


def _consts():
    ident = np.eye(128, dtype=np.float32)
    slopes = (2.0 ** (-8.0 * np.arange(1, 9) / 8)).astype(np.float64)
    s = np.arange(128)[:, None]
    q = np.arange(128)[None, :]
    biasT = np.zeros((2, 128, 2, 2, 2, 128), np.float32)
    for kh in range(2):
        for g in range(4):
            sl = slopes[kh * 4 + g]
            d0 = q + 128 - s
            d1 = q - s
            biasT[kh, :, g % 2, 0, g // 2, :] = np.where((d0 >= 0) & (d0 < 128), -sl * d0, NEG)
            biasT[kh, :, g % 2, 1, g // 2, :] = np.where((d1 >= 0) & (d1 < 128), -sl * d1, NEG)
    fix = np.ones((128, 4, 16), np.float32)
    for g, w in enumerate((2, 4, 8, 16)):
        t = np.arange(16)
        fix[:, g, :] = (w / np.minimum(t + 1, w))[None, :]
    return ident, biasT.reshape(2, 128, 1024), fix.reshape(128, 64)


def prep_shared(inp):
    f = lambda a: np.ascontiguousarray(a, dtype=np.float32)
    sh = {}
    sh["w_ada"] = f(inp["w_ada"])
    sh["b_adaT"] = f(inp["b_ada"].reshape(NL, 6, 8, 128).transpose(0, 3, 1, 2).reshape(NL, 128, 48))
    gains = np.zeros((NL, 128, 32), np.float32)
    gains[:, :, 0:8] = inp["norm1_g"].reshape(NL, 8, 128).transpose(0, 2, 1)
    gains[:, :, 8:16] = inp["norm2_g"].reshape(NL, 8, 128).transpose(0, 2, 1)
    gains[:, :, 16:24] = inp["mix_norm_g"].reshape(NL, 8, 128).transpose(0, 2, 1)
    gains[:, :, 24:28] = inp["pool_scale"].reshape(NL, 4, 128).transpose(0, 2, 1)
    gains[:, :, 28] = np.tile(inp["q_norm_g"], (1, 2))
    gains[:, :, 29] = np.tile(inp["k_norm_g"], (1, 2))
    sh["gains"] = gains
    sh["sinks"] = f(np.broadcast_to(inp["attn_sinks"][:, None, :], (NL, 128, 8)))
    w_in = inp["w_in"]
    ext = np.concatenate([w_in[:, :, 0:1024], w_in[:, :, 1024:1088], w_in[:, :, 1024:1088], w_in[:, :, 1088:1152], w_in[:, :, 1088:1152],
                          w_in[:, :, 1152:1280]], axis=2)
    chunkify = lambda w, noc: f(w.reshape(NL, 8, 128, noc, 128).transpose(0, 3, 2, 1, 4).reshape(NL, noc, 128, 1024))
    sh["w_in_c"] = chunkify(ext, 11)
    sh["pool_w"] = f(inp["pool_w"])
    sh["w_out_c"] = chunkify(inp["w_out"], 8)
    sh["wq_c"] = chunkify(inp["peer_wq"], 16)
    sh["skT"] = f(inp["peer_subkeys"].transpose(0, 1, 3, 2))
    sh["uT"] = f(inp["peer_u"].reshape(NL, 128, 128, 8, 128).transpose(0, 1, 4, 3, 2).reshape(NL, 128, 128, 1024))
    sh["peer_v"] = f(inp["peer_v"])
    ident, biasT, fix = _consts()
    sh["ident"] = ident
    sh["biasT"] = biasT
    sh["poolfix"] = fix
    return sh


def prep_core(inp, b):
    return {"xT": np.ascontiguousarray(inp["x"][b].T, dtype=np.float32),
            "cT": np.ascontiguousarray(inp["c"][b].reshape(8, 128).T, dtype=np.float32)}


def kernel(**inputs):
    inp = {k: np.asarray(v) for k, v in inputs.items()}
    nb = inp["x"].shape[0]
    sh = prep_shared(inp)
    in_maps = []
    for b in range(nb):
        m = dict(sh)
        m.update(prep_core(inp, b))
        in_maps.append(m)
    nc = build()
    res = run_bass_kernel_spmd(nc, in_maps, core_ids=list(range(nb)))
    out = np.stack([np.asarray(res.results[b]["oT"]).T for b in range(nb)], axis=0)
    return np.ascontiguousarray(out.astype(np.float32))
```

```python
import numpy as np
from contextlib import ExitStack
import concourse.bass as bass
import concourse.mybir as mybir
from concourse.bass_utils import run_bass_kernel_spmd

F32 = mybir.dt.float32
BF16 = mybir.dt.bfloat16
AF = mybir.ActivationFunctionType
ALU = mybir.AluOpType
AX = mybir.AxisListType

NL = 4
S = 2048
D = 1024
EPS = 1e-6
NEG = -30000.0
THR_EPS = 4e-6


class Buf:
    __slots__ = ("name", "w", "r")

    def __init__(self, name=""):
        self.name = name
        self.w = None
        self.r = []


class Op:
    __slots__ = ("eng", "fn", "deps", "needs_inc", "ev", "is_dma", "tag", "idx")


class Prog:
    ENGS = ("pe", "act", "dve", "pool", "sp")

    def __init__(self, nc, stack):
        self.nc = nc
        self.stack = stack
        self.ops = []
        self.eng_obj = {"pe": nc.tensor, "act": nc.scalar, "dve": nc.vector,
                        "pool": nc.gpsimd, "sp": nc.sync}
        self.nsem = 0
        self.bufs = []
        self.last = {}
        self.dmas = []
        self.emitted = 0
        self.eng_sem = {}
        self.eng_cnt = {}
        self.tag_sem = {}
        self.tag_cnt = {}
        self.waited = {e: {} for e in self.ENGS}
        self.n_ops = 0

    def buf(self, name=""):
        b = Buf(name)
        self.bufs.append(b)
        return b

    def drop(self, bufs):
        s = set(id(b) for b in bufs)
        self.bufs = [b for b in self.bufs if id(b) not in s]

    def op(self, eng, fn, reads=(), writes=(), dma_tag=None, extra_deps=()):
        o = Op()
        o.eng = eng
        o.fn = fn
        o.needs_inc = False
        o.ev = None
        o.is_dma = dma_tag is not None
        o.tag = dma_tag
        o.idx = self.n_ops
        self.n_ops += 1
        deps = list(extra_deps)
        for b in reads:
            if b.w is not None:
                deps.append(b.w)
        for b in writes:
            if b.w is not None:
                deps.append(b.w)
            deps.extend(b.r)
        seen = set()
        dd = []
        for d in deps:
            if eng == "pe" and d.eng == "pe" and not d.is_dma:
                continue
            if d.idx not in seen and d is not o:
                seen.add(d.idx)
                dd.append(d)
        o.deps = dd
        for d in dd:
            d.needs_inc = True
        for b in reads:
            if not o.is_dma:
                b.r = [r for r in b.r if r.is_dma or r.eng != eng]
            b.r.append(o)
        for b in writes:
            b.w = o
            b.r = []
        self.ops.append(o)
        self.last[eng] = o
        if o.is_dma:
            self.dmas.append(o)
        return o

    def barrier(self):
        deps = list(self.last.values()) + list(self.dmas)
        self.dmas = []
        for eng in self.ENGS:
            self.op(eng, lambda e: e.nop(), extra_deps=deps)

    def new_sem(self, name):
        self.nsem += 1
        return self.stack.enter_context(self.nc.semaphore(f"{name}_{self.nsem}"))

    def flush(self, final_wait_ops=()):
        LIM = 30000
        for o in final_wait_ops:
            o.needs_inc = True
        for b in self.bufs:
            if b.w is not None:
                b.w.needs_inc = True
            for r in b.r:
                r.needs_inc = True
        for o in self.last.values():
            o.needs_inc = True
        for o in self.ops:
            e = self.eng_obj[o.eng]
            need = {}
            for d in o.deps:
                sem, val = d.ev
                k = id(sem)
                if k not in need or need[k][1] < val:
                    need[k] = (sem, val)
            w = self.waited[o.eng]
            for k, (sem, val) in need.items():
                cur = w.get(k)
                if cur is None or cur < val:
                    e.wait_ge(sem, val)
                    w[k] = val
            ins = o.fn(e)
            if o.is_dma:
                t = (o.eng, o.tag)
                if t not in self.tag_sem or self.tag_cnt[t] + 16 > LIM:
                    self.tag_sem[t] = self.new_sem("d" + o.eng + str(o.tag))
                    self.tag_cnt[t] = 0
                self.tag_cnt[t] += 16
                ins.then_inc(self.tag_sem[t], 16)
                o.ev = (self.tag_sem[t], self.tag_cnt[t])
            elif o.needs_inc:
                if o.eng not in self.eng_sem or self.eng_cnt[o.eng] + 1 > LIM:
                    self.eng_sem[o.eng] = self.new_sem("e" + o.eng)
                    self.eng_cnt[o.eng] = 0
                self.eng_cnt[o.eng] += 1
                ins.then_inc(self.eng_sem[o.eng], 1)
                o.ev = (self.eng_sem[o.eng], self.eng_cnt[o.eng])
            o.fn = None
        self.ops = []
        fw = {}
        for o in final_wait_ops:
            sem, val = o.ev
            if id(sem) not in fw or fw[id(sem)][1] < val:
                fw[id(sem)] = (sem, val)
        for sem, val in fw.values():
            self.nc.sync.wait_ge(sem, val)


class _Stop(Exception):
    pass


def V(base, extra, dims):
    return bass.AP(base.tensor, base.offset + extra, [list(base.ap[0])] + [list(d) for d in dims])


def build(nl=NL, dbg=(), stop_after=None, ngroups=8):
    nc = bass.Bass("TRN2", target_bir_lowering=False)

    def din(name, shape):
        return nc.dram_tensor(name, shape, F32, kind="ExternalInput").ap()

    xT_d = din("xT", [1024, 2048])
    cT_d = din("cT", [128, 8])
    wada_d = din("w_ada", [NL, 1024, 6144])
    bada_d = din("b_adaT", [NL, 128, 48])
    gains_d = din("gains", [NL, 128, 32])
    sinks_d = din("sinks", [NL, 128, 8])
    win_d = din("w_in_c", [NL, 11, 128, 1024])
    poolw_d = din("pool_w", [NL, 4, 128, 128])
    wout_d = din("w_out_c", [NL, 8, 128, 1024])
    wq_d = din("wq_c", [NL, 16, 128, 1024])
    skT_d = din("skT", [NL, 2, 128, 128])
    uT_d = din("uT", [NL, 128, 128, 1024])
    v_d = din("peer_v", [NL, 16384, 1024])
    ident_d = din("ident", [128, 128])
    biasT_d = din("biasT", [2, 128, 1024])
    fix_d = din("poolfix", [128, 64])
    oT_d = nc.dram_tensor("oT", [1024, 2048], F32, kind="ExternalOutput").ap()
    cu_d = nc.dram_tensor("cache_u", [128, 128, 1024], BF16, kind="Internal").ap()
    cv_d = nc.dram_tensor("cache_v", [128, 128, 1024], BF16, kind="Internal").ap()
    dbg_d = {}
    for name, shape in dbg:
        dbg_d[name] = nc.dram_tensor("dbg_" + name, list(shape), F32, kind="ExternalOutput").ap()
    dbg_ops = []

    with ExitStack() as st:
        P = Prog(nc, st)

        uniq = [0]

        def sb(stack, name, shape, dt=F32):
            uniq[0] += 1
            return stack.enter_context(nc.sbuf_tensor(f"s{uniq[0]}_{name}", list(shape), dt))

        xT = sb(st, "xT", [128, 8, 2048])
        identf = sb(st, "identf", [128, 128])
        identb = sb(st, "identb", [128, 128], BF16)
        onesb = sb(st, "onesb", [128, 128], BF16)
        bdb = sb(st, "bdb", [128, 128], BF16)
        biasT = sb(st, "biasT", [128, 2, 1024])
        fix = sb(st, "fix", [128, 4, 16])
        cT = sb(st, "cT", [128, 8])
        condb = sb(st, "condb", [128, 8], BF16)
        modT = sb(st, "modT", [128, 48])
        badaT = sb(st, "badaT", [128, 48])
        gains = sb(st, "gains", [128, 32])
        esink = sb(st, "esink", [128, 8])
        a1 = sb(st, "a1", [128, 8])
        a2 = sb(st, "a2", [128, 8])
        qg8 = sb(st, "qg8", [128, 1])
        ps = st.enter_context(nc.psum_tensor("ps", [128, 8, 512], F32))

        BX = [[P.buf(f"x{k}_{g}") for g in range(8)] for k in range(8)]
        PB = [P.buf(f"psb{b}") for b in range(8)]
        B_const = P.buf("const")
        B_mod = P.buf("mod")
        B_small = P.buf("small")
        B_dbg = P.buf("dbg")

        def bx(k, t0, t1):
            return [BX[k][g] for g in range(t0 // 256, (t1 + 255) // 256)]

        def dump(name, src_ap, reads):
            if name in dbg_d:
                dst = dbg_d[name]
                if len(src_ap.shape) == 3 and src_ap.shape[1] * src_ap.shape[2] > 4096:
                    for k in range(src_ap.shape[1]):
                        dbg_ops.append(P.op("pool", lambda e, k=k: e.dma_start(out=dst[:, k, :], in_=src_ap[:, k, :]), reads=reads, writes=[B_dbg], dma_tag="dbg"))
                else:
                    dbg_ops.append(P.op("pool", lambda e: e.dma_start(out=dst, in_=src_ap), reads=reads, writes=[B_dbg], dma_tag="dbg"))

        for k in range(8):
            P.op("sp", lambda e, k=k: e.dma_start(out=xT[:, k, :], in_=xT_d[k * 128:(k + 1) * 128, :]),
                 writes=BX[k], dma_tag=f"x{k}")
        P.op("sp", lambda e: e.dma_start(out=identf[:], in_=ident_d), writes=[B_const], dma_tag="cc")
        P.op("pool", lambda e: e.dma_start(out=identb[:], in_=ident_d), writes=[B_const], dma_tag="cc")
        P.op("sp", lambda e: e.dma_start(out=biasT[:], in_=biasT_d.rearrange("k p n -> p k n")), writes=[B_const], dma_tag="cc")
        P.op("sp", lambda e: e.dma_start(out=fix[:].rearrange("p g n -> p (g n)"), in_=fix_d), writes=[B_const], dma_tag="cc")
        P.op("sp", lambda e: e.dma_start(out=cT[:], in_=cT_d), writes=[B_const], dma_tag="cc")
        P.op("dve", lambda e: e.memset(onesb[:], 1.0), writes=[B_const])
        P.op("dve", lambda e: e.memset(bdb[:], 0.0), writes=[B_const])
        P.op("dve", lambda e: e.memset(bdb[0:64, 0:64], 1.0), writes=[B_const])
        P.op("dve", lambda e: e.memset(bdb[64:128, 64:128], 1.0), writes=[B_const])
        P.op("act", lambda e: e.activation(out=condb[:], in_=cT[:], func=AF.Silu), reads=[B_const], writes=[B_const])

        def norm_mod(stk, xlo, n, a_t, sh_col0, out_ap_fn, out_bufs_fn, bank, tmp_tag):
            sqb = [sb(stk, f"sqb{tmp_tag}{i}", [128, n], BF16) for i in range(2)]
            rs = sb(stk, f"rs{tmp_tag}", [128, n])
            tmp = [sb(stk, f"nt{tmp_tag}{i}", [128, n]) for i in range(2)]
            Bsq = [P.buf() for _ in range(2)]
            Brs = P.buf()
            Btmp = [P.buf() for _ in range(2)]

            def run(t0):
                for k in range(8):
                    P.op("act", lambda e, k=k: e.activation(out=sqb[k % 2][:], in_=xT[:, k, t0:t0 + n], func=AF.Square),
                         reads=bx(k, t0, t0 + n), writes=[Bsq[k % 2]])
                    P.op("pe", lambda e, k=k: e.matmul(ps[:, bank, 0:n], lhsT=onesb[:], rhs=sqb[k % 2][:], start=(k == 0), stop=(k == 7)),
                         reads=[Bsq[k % 2], B_const], writes=[PB[bank]])
                P.op("act", lambda e: e.activation(out=rs[:], in_=ps[:, bank, 0:n], func=AF.Ln, scale=1.0 / D, bias=EPS),
                     reads=[PB[bank]], writes=[Brs])
                P.op("act", lambda e: e.activation(out=rs[:], in_=rs[:], func=AF.Exp, scale=-0.5), reads=[Brs], writes=[Brs])
                for k in range(8):
                    P.op("dve", lambda e, k=k: e.scalar_tensor_tensor(out=tmp[k % 2][:], in0=xT[:, k, t0:t0 + n], scalar=a_t[:, k:k + 1],
                                                                      in1=rs[:], op0=ALU.mult, op1=ALU.mult),
                         reads=bx(k, t0, t0 + n) + [Brs, B_mod], writes=[Btmp[k % 2]])
                    P.op("act", lambda e, k=k: e.activation(out=out_ap_fn(k, t0), in_=tmp[k % 2][:], func=AF.Identity,
                                                            bias=modT[:, sh_col0 + k:sh_col0 + k + 1], scale=1.0),
                         reads=[Btmp[k % 2], B_mod], writes=out_bufs_fn(k, t0))
            return run

        stopped = [False]

        def chk(name, l):
            if stop_after == (name, l):
                P.barrier()
                P.flush()
                raise _Stop()

        try:
          for l in range(nl):
              with ExitStack() as sa:
                  wada = [sb(sa, f"wada{i}", [128, 8, 1024], BF16) for i in range(2)]
                  Bw = [P.buf() for _ in range(2)]
                  P.op("sp", lambda e, l=l: e.dma_start(out=badaT[:], in_=bada_d[l]), writes=[B_mod], dma_tag="cm")
                  P.op("sp", lambda e, l=l: e.dma_start(out=gains[:], in_=gains_d[l]), writes=[B_mod], dma_tag="cm")
                  P.op("sp", lambda e, l=l: e.dma_start(out=esink[:], in_=sinks_d[l]), writes=[B_mod], dma_tag="cm")
                  for m in range(6):
                      P.op("pool", lambda e, m=m, l=l: e.dma_start(
                          out=wada[m % 2][:], in_=wada_d[l][:, m * 1024:(m + 1) * 1024].rearrange("(k p) n -> p k n", p=128)),
                          writes=[Bw[m % 2]], dma_tag=f"wa{m % 2}")

                      def mm_ada(e, m=m):
                          ins = None
                          for j in range(8):
                              for k in range(8):
                                  ins = e.matmul(ps[:, 0, m * 8 + j:m * 8 + j + 1], lhsT=wada[m % 2][:, k, j * 128:(j + 1) * 128],
                                                 rhs=condb[:, k:k + 1], start=(k == 0), stop=(k == 7))
                          return ins
                      P.op("pe", mm_ada, reads=[Bw[m % 2], B_const], writes=[PB[0]])
                  P.op("dve", lambda e: e.tensor_tensor(out=modT[:], in0=ps[:, 0, 0:48], in1=badaT[:], op=ALU.add),
                       reads=[PB[0], B_mod], writes=[B_mod])
                  P.op("dve", lambda e: e.scalar_tensor_tensor(out=a1[:], in0=modT[:, 8:16], scalar=1.0, in1=gains[:, 0:8], op0=ALU.add, op1=ALU.mult),
                       reads=[B_mod], writes=[B_mod])
                  P.op("dve", lambda e: e.scalar_tensor_tensor(out=a2[:], in0=modT[:, 32:40], scalar=1.0, in1=gains[:, 8:16], op0=ALU.add, op1=ALU.mult),
                       reads=[B_mod], writes=[B_mod])
                  P.op("act", lambda e: e.activation(out=esink[:], in_=esink[:], func=AF.Exp), reads=[B_mod], writes=[B_mod])
                  P.op("dve", lambda e: e.tensor_scalar(out=qg8[:], in0=gains[:, 28:29], scalar1=0.125, scalar2=None, op0=ALU.mult),
                       reads=[B_mod], writes=[B_mod])
                  dump(f"mod{l}", modT[:], [B_mod])
                  P.barrier()
                  P.flush()
                  P.drop(Bw)
              if stop_after == ("ada", l):
                  break

              fill_ops = []
              fill_next = [0]

              def fill_some(n, l=l):
                  for _ in range(n):
                      c = fill_next[0]
                      if c >= 128:
                          return
                      fill_next[0] += 1
                      fill_ops.append(P.op("pool", lambda e, c=c: e.dma_start(out=cu_d[c], in_=uT_d[l, c]), dma_tag="fu"))
                      fill_ops.append(P.op("pool", lambda e, c=c: e.dma_start(out=cv_d[c], in_=v_d[l, c * 128:(c + 1) * 128, :]), dma_tag="fv"))

              with ExitStack() as s1:
                  try:
                      hT = sb(s1, "hT", [128, 8, 2048], BF16)
                      BH = [[P.buf() for _ in range(4)] for _ in range(8)]
                      ypr = sb(s1, "ypr", [128, 4, 2048], BF16)
                      BYP = [[P.buf() for _ in range(4)] for _ in range(4)]
                      pT = sb(s1, "pT", [128, 2048])
                      tA = sb(s1, "tA", [128, 2048])
                      tB = sb(s1, "tB", [128, 2048])
                      pooled = sb(s1, "pooled", [128, 2048], BF16)
                      B_pT, B_tA, B_tB, B_pooled = P.buf(), P.buf(), P.buf(), P.buf()
                      qnT = sb(s1, "qnT", [128, 4, 2048], BF16)
                      knT = sb(s1, "knT", [128, 2, 2048], BF16)
                      BQ = [[P.buf() for _ in range(4)] for _ in range(4)]
                      BK = [[P.buf() for _ in range(4)] for _ in range(2)]
                      v_sb = sb(s1, "v_sb", [128, 16, 2, 65], BF16)
                      BV = [P.buf() for _ in range(16)]
                      wch = [sb(s1, f"wch{i}", [128, 8, 128], BF16) for i in range(2)]
                      BW = [P.buf() for _ in range(2)]
                      poolw = sb(s1, "poolw", [128, 4, 128], BF16)
                      B_pw = P.buf()
                      local_bufs = [b for row in BH for b in row] + [b for row in BYP for b in row] + [B_pT, B_tA, B_tB, B_pooled] + \
                          [b for row in BQ for b in row] + [b for row in BK for b in row] + BV + BW + [B_pw]
                      wslot = [0]

                      def load_w(src_ap):
                          i = wslot[0] % 2
                          wslot[0] += 1
                          P.op("pool", lambda e: e.dma_start(out=wch[i][:].rearrange("p k n -> p (k n)"), in_=src_ap), writes=[BW[i]], dma_tag=f"w{i}")
                          fill_some(4)
                          return i

                      P.op("pool", lambda e, l=l: e.dma_start(out=poolw[:], in_=poolw_d[l].rearrange("g c d -> c g d")), writes=[B_pw], dma_tag="pw")
                      P.op("dve", lambda e: e.memset(v_sb[:, :, :, 64:65], 1.0), writes=BV)

                      nm = norm_mod(s1, 0, 512, a1, 0, lambda k, t0: hT[:, k, t0:t0 + 512], lambda k, t0: [BH[k][t0 // 512]], 1, "a")
                      for tg in range(4):
                          nm(tg * 512)

                      dump(f"h{l}", hT[:], [b for row in BH for b in row])
                      chk("norm", l)
                      qraw = sb(s1, "qraw", [128, 512])
                      qsq = sb(s1, "qsq", [128, 512], BF16)
                      qrs = sb(s1, "qrs", [128, 512])
                      B_qraw, B_qsq, B_qrs = P.buf(), P.buf(), P.buf()
                      local_bufs += [B_qraw, B_qsq, B_qrs]
                      pbank = [0]
                      for oc in range(4, 10):
                          wi = load_w(win_d[l, oc])
                          for tg in range(4):
                              bk = 2 + (pbank[0] % 2)
                              pbank[0] += 1

                              def mmq(e, wi=wi, tg=tg, bk=bk):
                                  ins = None
                                  for k in range(8):
                                      ins = e.matmul(ps[:, bk, :], lhsT=wch[wi][:, k, :], rhs=hT[:, k, tg * 512:(tg + 1) * 512], start=(k == 0), stop=(k == 7))
                                  return ins
                              P.op("pe", mmq, reads=[BW[wi]] + [BH[k][tg] for k in range(8)], writes=[PB[bk]])
                              P.op("act", lambda e, bk=bk: e.activation(out=qraw[:], in_=ps[:, bk, :], func=AF.Copy), reads=[PB[bk]], writes=[B_qraw])
                              P.op("act", lambda e: e.activation(out=qsq[:], in_=qraw[:], func=AF.Square), reads=[B_qraw], writes=[B_qsq])
                              P.op("pe", lambda e: e.matmul(ps[:, 1, :], lhsT=bdb[:], rhs=qsq[:], start=True, stop=True), reads=[B_qsq, B_const], writes=[PB[1]])
                              P.op("act", lambda e: e.activation(out=qrs[:], in_=ps[:, 1, :], func=AF.Ln, scale=1.0 / 64, bias=EPS), reads=[PB[1]], writes=[B_qrs])
                              P.op("act", lambda e: e.activation(out=qrs[:], in_=qrs[:], func=AF.Exp, scale=-0.5), reads=[B_qrs], writes=[B_qrs])
                              if oc < 8:
                                  dst, dbuf, gcol = qnT[:, oc - 4, tg * 512:(tg + 1) * 512], BQ[oc - 4][tg], qg8[:, 0:1]
                              else:
                                  dst, dbuf, gcol = knT[:, oc - 8, tg * 512:(tg + 1) * 512], BK[oc - 8][tg], gains[:, 29:30]
                              P.op("dve", lambda e, dst=dst, gcol=gcol: e.scalar_tensor_tensor(out=dst, in0=qraw[:], scalar=gcol, in1=qrs[:], op0=ALU.mult, op1=ALU.mult),
                                   reads=[B_qraw, B_qrs, B_mod], writes=[dbuf])

                      dump(f"qn{l}", qnT[:], [b for row in BQ for b in row])
                      dump(f"kn{l}", knT[:], [b for row in BK for b in row])
                      chk("qk", l)
                      wi = load_w(win_d[l, 10])
                      for tt in range(16):
                          bk = 2 + (pbank[0] % 2)
                          pbank[0] += 1

                          def mmv(e, wi=wi, tt=tt, bk=bk):
                              ins = None
                              for k in range(8):
                                  ins = e.matmul(ps[:, bk, 0:128], lhsT=hT[:, k, tt * 128:(tt + 1) * 128], rhs=wch[wi][:, k, :], start=(k == 0), stop=(k == 7))
                              return ins
                          P.op("pe", mmv, reads=[BW[wi]] + [BH[k][tt // 4] for k in range(8)], writes=[PB[bk]])
                          P.op("act", lambda e, tt=tt, bk=bk: e.activation(out=v_sb[:, tt, :, 0:64], in_=ps[:, bk, 0:128].rearrange("p (a b) -> p a b", a=2), func=AF.Copy),
                               reads=[PB[bk]], writes=[BV[tt]])

                      dump(f"v{l}", v_sb[:], BV)
                      chk("v", l)
                      for g in range(4):
                          w = (2, 4, 8, 16)[g]
                          wi = load_w(win_d[l, g])
                          for tg in range(4):
                              bk = 2 + (pbank[0] % 2)
                              pbank[0] += 1

                              def mmp(e, wi=wi, tg=tg, bk=bk):
                                  ins = None
                                  for k in range(8):
                                      ins = e.matmul(ps[:, bk, :], lhsT=wch[wi][:, k, :], rhs=hT[:, k, tg * 512:(tg + 1) * 512], start=(k == 0), stop=(k == 7))
                                  return ins
                              P.op("pe", mmp, reads=[BW[wi]] + [BH[k][tg] for k in range(8)], writes=[PB[bk]])
                              P.op("act", lambda e, tg=tg, bk=bk: e.activation(out=pT[:, tg * 512:(tg + 1) * 512], in_=ps[:, bk, :], func=AF.Copy), reads=[PB[bk]], writes=[B_pT])
                          src, Bsrc = pT, B_pT
                          bufs2 = [(tA, B_tA), (tB, B_tB)]
                          sh = 1
                          step = 0
                          while sh < w:
                              dst, Bdst = bufs2[step % 2]
                              eng = "dve" if step % 2 == 0 else "pool"
                              P.op(eng, lambda e, dst=dst, src=src, sh=sh: e.tensor_tensor(out=dst[:, sh:], in0=src[:, sh:], in1=src[:, :S - sh], op=ALU.add),
                                   reads=[Bsrc], writes=[Bdst])
                              P.op(eng, lambda e, dst=dst, src=src, sh=sh: e.tensor_copy(out=dst[:, 0:sh], in_=src[:, 0:sh]), reads=[Bsrc], writes=[Bdst])
                              src, Bsrc = dst, Bdst
                              sh *= 2
                              step += 1
                          P.op("dve", lambda e, src=src, g=g: e.tensor_tensor(out=src[:, 0:16], in0=src[:, 0:16], in1=fix[:, g, :], op=ALU.mult),
                               reads=[Bsrc, B_const], writes=[Bsrc])
                          P.op("dve", lambda e, src=src, w=w: e.scalar_tensor_tensor(out=pooled[:], in0=src[:], scalar=1.0 / w, in1=pT[:], op0=ALU.mult, op1=ALU.subtract),
                               reads=[Bsrc, B_pT], writes=[B_pooled])
                          for tg in range(4):
                              bk = 2 + (pbank[0] % 2)
                              pbank[0] += 1
                              P.op("pe", lambda e, g=g, tg=tg, bk=bk: e.matmul(ps[:, bk, :], lhsT=poolw[:, g, :], rhs=pooled[:, tg * 512:(tg + 1) * 512], start=True, stop=True),
                                   reads=[B_pw, B_pooled], writes=[PB[bk]])
                              P.op("act", lambda e, g=g, tg=tg, bk=bk: e.activation(out=ypr[:, g, tg * 512:(tg + 1) * 512], in_=ps[:, bk, :], func=AF.Identity,
                                                                                 scale=gains[:, 24 + g:25 + g]),
                                   reads=[PB[bk], B_mod], writes=[BYP[g][tg]])

                      dump(f"ypr{l}", ypr[:], [b for row in BYP for b in row])
                      chk("pool", l)
                      prs, B_prs = qrs, B_qrs
                      for tg in range(4):
                          for g in range(4):
                              P.op("act", lambda e, g=g, tg=tg: e.activation(out=qsq[:], in_=ypr[:, g, tg * 512:(tg + 1) * 512], func=AF.Square),
                                   reads=[BYP[g][tg]], writes=[B_qsq])
                              P.op("pe", lambda e, g=g: e.matmul(ps[:, 1, :], lhsT=onesb[:], rhs=qsq[:], start=(g == 0), stop=(g == 3)),
                                   reads=[B_qsq, B_const], writes=[PB[1]])
                          P.op("act", lambda e: e.activation(out=prs[:], in_=ps[:, 1, :], func=AF.Ln, scale=1.0 / 512, bias=EPS), reads=[PB[1]], writes=[B_prs])
                          P.op("act", lambda e: e.activation(out=prs[:], in_=prs[:], func=AF.Exp, scale=-0.5), reads=[B_prs], writes=[B_prs])
                          for g in range(4):
                              P.op("dve", lambda e, g=g, tg=tg: e.scalar_tensor_tensor(out=hT[:, g, tg * 512:(tg + 1) * 512], in0=ypr[:, g, tg * 512:(tg + 1) * 512],
                                                                                   scalar=gains[:, 16 + g:17 + g], in1=prs[:], op0=ALU.mult, op1=ALU.mult),
                                   reads=[BYP[g][tg], B_prs, B_mod], writes=[BH[g][tg]])

                      chk("poolnorm", l)
                      ssb = sb(s1, "ssb", [128, 1024])
                      PT = sb(s1, "PT", [128, 1024], BF16)
                      B_ssb, B_PT = P.buf(), P.buf()
                      den = sb(s1, "den", [128, 8])
                      yatt = sb(s1, "yatt", [128, 8, 64])
                      ssq = sb(s1, "ssq", [128, 2])
                      yab = sb(s1, "yab", [128, 512], BF16)
                      B_den, B_yatt, B_ssq, B_yab = P.buf(), P.buf(), P.buf(), P.buf()
                      yjunk, B_yjunk = ssb, B_ssb
                      local_bufs += [B_ssb, B_PT, B_den, B_yatt, B_ssq, B_yab]
                      for n in range(16):
                          fill_some(4)
                          js = [1] if n == 0 else [0, 1]
                          for kh in range(2):
                              def mms(e, n=n, kh=kh):
                                  ins = None
                                  for j in range(2):
                                      kb = max(n - 1 + j, 0)
                                      for g in range(4):
                                          h = kh * 4 + g
                                          r = h % 2
                                          lo = r * 64
                                          col = (j * 2 + g // 2) * 128
                                          ins = e.matmul(ps[:, 4 + r, col:col + 128], lhsT=knT[lo:lo + 64, kh, kb * 128:(kb + 1) * 128],
                                                         rhs=qnT[lo:lo + 64, h // 2, n * 128:(n + 1) * 128], start=True, stop=True)
                                  return ins
                              rd = [BK[kh][(n - 1) // 4 if n > 0 else 0], BK[kh][n // 4], BQ[kh * 2][n // 4], BQ[kh * 2 + 1][n // 4]]
                              P.op("pe", mms, reads=rd, writes=[PB[4], PB[5]])
                              P.op("dve", lambda e, kh=kh: e.tensor_tensor(out=ssb[:], in0=V(ps[:, 4, :], 0, [[1, 1024]]),
                                                                          in1=biasT[:, kh, :], op=ALU.add),
                                   reads=[PB[4], PB[5], B_const], writes=[B_ssb])
                              P.op("act", lambda e: e.activation(out=PT[:], in_=ssb[:], func=AF.Exp), reads=[B_ssb], writes=[B_PT])

                              def mmo(e, n=n, kh=kh, js=js):
                                  ins = None
                                  for g in range(4):
                                      for j in js:
                                          kb = n - 1 + j
                                          col = (g % 2) * 512 + (j * 2 + g // 2) * 128
                                          ins = e.matmul(ps[:, 6 + kh, g * 65:(g + 1) * 65], lhsT=PT[:, col:col + 128],
                                                         rhs=v_sb[:, kb, kh, :], start=(j == js[0]), stop=(j == 1))
                                  return ins
                              P.op("pe", mmo, reads=[B_PT, BV[max(n - 1, 0)], BV[n]], writes=[PB[6 + kh]])
                          for kh in range(2):
                              P.op("dve", lambda e, kh=kh: e.tensor_tensor(out=den[:, kh * 4:(kh + 1) * 4], in0=V(ps[:, 6 + kh, :], 64, [[65, 4]]),
                                                                          in1=esink[:, kh * 4:(kh + 1) * 4], op=ALU.add),
                                   reads=[PB[6 + kh], B_mod], writes=[B_den])
                          P.op("dve", lambda e: e.reciprocal(out=den[:], in_=den[:]), reads=[B_den], writes=[B_den])
                          for kh in range(2):
                              P.op("dve", lambda e, kh=kh: e.tensor_tensor(out=yatt[:, kh * 4:(kh + 1) * 4, :], in0=V(ps[:, 6 + kh, :], 0, [[65, 4], [1, 64]]),
                                                                          in1=V(den[:], kh * 4, [[1, 4], [0, 64]]), op=ALU.mult),
                                   reads=[PB[6 + kh], B_den], writes=[B_yatt])
                          P.op("act", lambda e: e.activation(out=yjunk[:, 0:512], in_=yatt[:].rearrange("p a b -> p (a b)"), func=AF.Square, accum_out=ssq[:, 0:1]),
                               reads=[B_yatt], writes=[B_ssq, B_yjunk])
                          P.op("act", lambda e: e.activation(out=ssq[:, 1:2], in_=ssq[:, 0:1], func=AF.Ln, scale=1.0 / 512, bias=EPS), reads=[B_ssq], writes=[B_ssq])
                          P.op("act", lambda e: e.activation(out=ssq[:, 1:2], in_=ssq[:, 1:2], func=AF.Exp, scale=-0.5), reads=[B_ssq], writes=[B_ssq])
                          P.op("dve", lambda e: e.tensor_scalar(out=yab[:], in0=yatt[:].rearrange("p a b -> p (a b)"), scalar1=ssq[:, 1:2], scalar2=None, op0=ALU.mult),
                               reads=[B_yatt, B_ssq], writes=[B_yab])
                          psb = ps[:, 1, :].bitcast(BF16)

                          def trn(e):
                              ins = None
                              for c in range(4):
                                  ins = e.transpose(psb[:, c * 128:(c + 1) * 128], yab[:, c * 128:(c + 1) * 128], identb[:])
                              return ins
                          P.op("pe", trn, reads=[B_yab, B_const], writes=[PB[1]])
                          for c in range(4):
                              P.op("act", lambda e, c=c, n=n: e.activation(out=hT[:, 4 + c, n * 128:(n + 1) * 128], in_=psb[:, c * 128:(c + 1) * 128], func=AF.Identity,
                                                                        scale=gains[:, 20 + c:21 + c]),
                                   reads=[PB[1], B_mod], writes=[BH[4 + c][n // 4]])
                      dump(f"y{l}", hT[:], [b for row in BH for b in row])

                      chk("attn", l)
                      for oc in range(8):
                          wi = load_w(wout_d[l, oc])
                          for tg in range(4):
                              bk = 2 + (pbank[0] % 2)
                              pbank[0] += 1

                              def mmw(e, wi=wi, tg=tg, bk=bk):
                                  ins = None
                                  for k in range(8):
                                      ins = e.matmul(ps[:, bk, :], lhsT=wch[wi][:, k, :], rhs=hT[:, k, tg * 512:(tg + 1) * 512], start=(k == 0), stop=(k == 7))
                                  return ins
                              P.op("pe", mmw, reads=[BW[wi]] + [BH[k][tg] for k in range(8)], writes=[PB[bk]])
                              P.op("dve", lambda e, oc=oc, tg=tg, bk=bk: e.scalar_tensor_tensor(out=xT[:, oc, tg * 512:(tg + 1) * 512], in0=ps[:, bk, :],
                                                                                              scalar=modT[:, 16 + oc:17 + oc], in1=xT[:, oc, tg * 512:(tg + 1) * 512],
                                                                                              op0=ALU.mult, op1=ALU.add),
                                   reads=[PB[bk], B_mod] + bx(oc, tg * 512, (tg + 1) * 512), writes=bx(oc, tg * 512, (tg + 1) * 512))
                      dump(f"x1_{l}", xT[:], [b for row in BX for b in row])
                      P.barrier()
                      P.flush()
                      P.drop(local_bufs)
                  except _Stop:
                      stopped[0] = True
              if stopped[0]:
                  break
              if stop_after == ("s1", l):
                  break

              with ExitStack() as s2:
                  try:
                      GT = sb(s2, "GT", [128, 256, 128], BF16)
                      B_GT = P.buf()
                      h2g = sb(s2, "h2g", [128, 8, 256], BF16)
                      B_h2 = [P.buf() for _ in range(8)]
                      NSL = 7
                      reg = sb(s2, "reg", [128, NSL * 2048], BF16)
                      qTg = reg[:, 0:4096].rearrange("p (a t) -> p a t", a=16)
                      B_qT = [P.buf() for _ in range(16)]

                      def rv(s_, i):
                          b0 = 4096 + s_ * 3584 + i * 512
                          return reg[:, b0:b0 + 512].rearrange("p (t n) -> p t n", t=4)
                      qrep = [[rv(s_, 0), rv(s_, 1)] for s_ in range(2)]
                      EQ = [rv(s_, 2) for s_ in range(2)]
                      Lm = [rv(s_, 3) for s_ in range(2)]
                      E2 = [rv(s_, 4) for s_ in range(2)]
                      Rm = [rv(s_, 5) for s_ in range(2)]
                      Mk = [rv(s_, 6) for s_ in range(2)]
                      B_qrep = [[P.buf(), P.buf()] for _ in range(2)]
                      B_EQ = [P.buf() for _ in range(2)]
                      B_L = [P.buf() for _ in range(2)]
                      B_E2 = [P.buf() for _ in range(2)]
                      B_R = [P.buf() for _ in range(2)]
                      B_M = [P.buf() for _ in range(2)]
                      ub = [reg[:, i * 2048:i * 2048 + 1024].rearrange("p (k n) -> p k n", k=8) for i in range(NSL)]
                      vb = [reg[:, i * 2048 + 1024:(i + 1) * 2048] for i in range(NSL)]
                      B_ub = [P.buf() for _ in range(NSL)]
                      B_vb = [P.buf() for _ in range(NSL)]
                      fdum = sb(s2, "fdum", [128, 2])
                      B_fd = P.buf()
                      region_bufs = B_qT + [b for r_ in B_qrep for b in r_] + B_EQ + B_L + B_E2 + B_R + B_M + B_ub + B_vb

                      def fence():
                          P.op("pool", lambda e: e.memset(fdum[:], 0.0), writes=region_bufs + [B_fd])

                      s_sb = sb(s2, "s_sb", [128, 16, 128])
                      wk = sb(s2, "wk", [128, 16, 128])
                      B_s, B_wk = P.buf(), P.buf()
                      top = sb(s2, "top", [128, 16, 16])
                      c16 = sb(s2, "c16", [128, 8, 16])
                      e16 = sb(s2, "e16", [128, 8, 16])
                      Zt = sb(s2, "Zt", [128, 8])
                      tme = sb(s2, "tme", [128, 8])
                      tok3 = sb(s2, "tok3", [128, 3, 128])
                      B_top, B_c16, B_e16, B_Z, B_tok3 = P.buf(), P.buf(), P.buf(), P.buf(), P.buf()
                      B_tophp = [P.buf() for _ in range(16)]
                      B_wkhp = [P.buf() for _ in range(16)]
                      B_c16h = [P.buf() for _ in range(8)]
                      slotT = sb(s2, "slotT", [128, 3, 256])
                      B_slot = P.buf()
                      skb = sb(s2, "skb", [128, 2, 128], BF16)
                      B_sk = P.buf()
                      gl = [sb(s2, f"gl{i}", [128, 256]) for i in range(2)]
                      actT = [sb(s2, f"actT{i}", [128, 256], BF16) for i in range(2)]
                      B_gl = [P.buf() for _ in range(2)]
                      B_act = [P.buf() for _ in range(2)]
                      wqc = [sb(s2, f"wqc{i}", [128, 8, 128], BF16) for i in range(2)]
                      B_wq = [P.buf() for _ in range(2)]
                      local_bufs = [B_GT, B_s, B_wk, B_top, B_c16, B_e16, B_Z, B_tok3, B_slot, B_sk, B_fd] + B_tophp + B_wkhp + B_c16h + \
                          B_h2 + region_bufs + B_gl + B_act + B_wq

                      P.op("pool", lambda e, l=l: e.dma_start(out=skb[:], in_=skT_d[l].rearrange("a d n -> d a n")), writes=[B_sk], dma_tag="sk")
                      nm2 = norm_mod(s2, 0, 256, a2, 24, lambda k, t0: h2g[:, k, :], lambda k, t0: [B_h2[k]], 7, "b")
                      cand = s_sb[:].rearrange("p a b -> p (a b)").rearrange("p (h n) -> p h n", h=8)
                      cwk = wk[:].rearrange("p a b -> p (a b)").rearrange("p (h n) -> p h n", h=8)

                      for G in range(ngroups):
                          t0 = G * 256
                          nm2(t0)
                          fence()
                          for hp in range(16):
                              i = hp % 2
                              P.op("pool", lambda e, i=i, hp=hp, l=l: e.dma_start(out=wqc[i][:].rearrange("p k n -> p (k n)"), in_=wq_d[l, hp]),
                                   writes=[B_wq[i]], dma_tag=f"wq{i}")
                              bk = 6 + (hp % 2)

                              def mmq2(e, i=i, bk=bk):
                                  ins = None
                                  for k in range(8):
                                      ins = e.matmul(ps[:, bk, 0:256], lhsT=wqc[i][:, k, :], rhs=h2g[:, k, :], start=(k == 0), stop=(k == 7))
                                  return ins
                              P.op("pe", mmq2, reads=[B_wq[i]] + B_h2, writes=[PB[bk]])
                              P.op("act", lambda e, hp=hp, bk=bk: e.activation(out=qTg[:, hp, :], in_=ps[:, bk, 0:256], func=AF.Copy), reads=[PB[bk]], writes=[B_qT[hp]])
                          for tt in range(2):
                              def mmsc(e, tt=tt):
                                  ins = None
                                  for hp in range(16):
                                      ins = e.matmul(ps[:, hp // 4, (hp % 4) * 128:(hp % 4 + 1) * 128], lhsT=qTg[:, hp, tt * 128:(tt + 1) * 128],
                                                     rhs=skb[:, hp % 2, :], start=True, stop=True)
                                  return ins
                              P.op("pe", mmsc, reads=B_qT + [B_sk], writes=PB[0:4])
                              for b4 in range(4):
                                  if b4 % 2 == 0:
                                      P.op("act", lambda e, b4=b4: e.activation(out=s_sb[:, b4 * 4:(b4 + 1) * 4, :].rearrange("p a b -> p (a b)"), in_=ps[:, b4, :], func=AF.Copy),
                                           reads=[PB[b4]], writes=[B_s])
                                  else:
                                      P.op("dve", lambda e, b4=b4: e.tensor_copy(out=s_sb[:, b4 * 4:(b4 + 1) * 4, :].rearrange("p a b -> p (a b)"), in_=ps[:, b4, :]),
                                           reads=[PB[b4]], writes=[B_s])
                              for hp in range(16):
                                  P.op("dve", lambda e, hp=hp: e.max(out=top[:, hp, 0:8], in_=s_sb[:, hp, :]), reads=[B_s], writes=[B_tophp[hp]])
                              for hp in range(16):
                                  P.op("dve", lambda e, hp=hp: e.match_replace(out=wk[:, hp, :], in_to_replace=top[:, hp, 0:8], in_values=s_sb[:, hp, :], imm_value=-1e30),
                                       reads=[B_s, B_tophp[hp]], writes=[B_wkhp[hp]])
                              for hp in range(16):
                                  P.op("dve", lambda e, hp=hp: e.max(out=top[:, hp, 8:16], in_=wk[:, hp, :]), reads=[B_wkhp[hp]], writes=[B_tophp[hp]])
                              P.op("dve", lambda e: e.tensor_tensor(out=cand.rearrange("p h (a b) -> p h a b", a=16),
                                                                    in0=V(top[:], 0, [[32, 8], [1, 16], [0, 16]]),
                                                                    in1=V(top[:], 16, [[32, 8], [0, 16], [1, 16]]), op=ALU.add),
                                   reads=B_tophp, writes=[B_s])
                              for h in range(8):
                                  P.op("dve", lambda e, h=h: e.max(out=c16[:, h, 0:8], in_=cand[:, h, :]), reads=[B_s], writes=[B_c16h[h]])
                              for h in range(8):
                                  P.op("dve", lambda e, h=h: e.match_replace(out=cwk[:, h, :], in_to_replace=c16[:, h, 0:8], in_values=cand[:, h, :], imm_value=-1e30),
                                       reads=[B_s, B_c16h[h]], writes=[B_wkhp[2 * h], B_wkhp[2 * h + 1]])
                              for h in range(8):
                                  P.op("dve", lambda e, h=h: e.max(out=c16[:, h, 8:16], in_=cwk[:, h, :]), reads=[B_wkhp[2 * h], B_wkhp[2 * h + 1]], writes=[B_c16h[h]])
                              P.op("dve", lambda e: e.tensor_tensor(out=e16[:], in0=c16[:], in1=V(c16[:], 0, [[16, 8], [0, 16]]), op=ALU.subtract),
                                   reads=B_c16h, writes=[B_e16])
                              P.op("act", lambda e: e.activation(out=e16[:], in_=e16[:], func=AF.Exp), reads=[B_e16], writes=[B_e16])
                              P.op("dve", lambda e: e.tensor_reduce(out=Zt[:], in_=e16[:], axis=AX.X, op=ALU.add), reads=[B_e16], writes=[B_Z])
                              P.op("dve", lambda e: e.reciprocal(out=Zt[:], in_=Zt[:]), reads=[B_Z], writes=[B_Z])
                              P.op("dve", lambda e: e.tensor_scalar(out=tme[:], in0=V(c16[:], 15, [[16, 8]]), scalar1=-THR_EPS, scalar2=None, op0=ALU.add),
                                   reads=B_c16h, writes=[B_Z])
                              top1v = V(top[:], 0, [[32, 8], [1, 16]])
                              P.op("dve", lambda e: e.tensor_copy(out=tok3[:, 0, :].rearrange("p (h a) -> p h a", h=8), in_=top1v), reads=B_tophp, writes=[B_tok3])
                              P.op("dve", lambda e: e.tensor_tensor(out=tok3[:, 1, :].rearrange("p (h a) -> p h a", h=8), in0=V(tme[:], 0, [[1, 8], [0, 16]]),
                                                                    in1=top1v, op=ALU.subtract), reads=B_tophp + [B_Z], writes=[B_tok3])
                              P.op("dve", lambda e: e.tensor_tensor(out=e16[:], in0=top1v, in1=V(c16[:], 0, [[16, 8], [0, 16]]), op=ALU.subtract),
                                   reads=B_tophp + B_c16h, writes=[B_e16])
                              P.op("act", lambda e: e.activation(out=e16[:], in_=e16[:], func=AF.Exp), reads=[B_e16], writes=[B_e16])
                              P.op("dve", lambda e: e.tensor_tensor(out=tok3[:, 2, :].rearrange("p (h a) -> p h a", h=8), in0=e16[:], in1=V(Zt[:], 0, [[1, 8], [0, 16]]), op=ALU.mult),
                                   reads=[B_e16, B_Z], writes=[B_tok3])

                              def trs(e):
                                  ins = None
                                  for i in range(3):
                                      ins = e.transpose(ps[:, 4, i * 128:(i + 1) * 128], tok3[:, i, :], identf[:])
                                  return ins
                              P.op("pe", trs, reads=[B_tok3, B_const], writes=[PB[4]])
                              P.op("act", lambda e, tt=tt: e.activation(out=slotT[:, :, tt * 128:(tt + 1) * 128], in_=ps[:, 4, 0:384].rearrange("p (a b) -> p a b", a=3), func=AF.Copy),
                                   reads=[PB[4]], writes=[B_slot])
                          if G == 0:
                              dump(f"slotT{l}", slotT[:], [B_slot])
                          NSB = 64

                          def stA(sbi):
                              tl = sbi * 4
                              s_ = sbi % 2
                              for p_ in range(2):
                                  P.op("act", lambda e, p_=p_: e.activation(out=qrep[s_][p_].rearrange("p t (h a) -> p t h a", h=8),
                                                                           in_=V(reg[:], p_ * 256 + tl, [[1, 4], [512, 8], [0, 16]]), func=AF.Copy),
                                       reads=B_qT, writes=[B_qrep[s_][p_]])

                                  def mmrep(e, p_=p_):
                                      ins = None
                                      for i in range(4):
                                          ins = e.matmul(ps[:, 2 * p_ + s_, i * 128:(i + 1) * 128], lhsT=qrep[s_][p_][:, i, :], rhs=skb[:, p_, :], start=True, stop=True)
                                      return ins
                                  P.op("pe", mmrep, reads=[B_qrep[s_][p_], B_sk], writes=[PB[2 * p_ + s_]])

                          def stB(sbi):
                              tl = sbi * 4
                              s_ = sbi % 2
                              ps1 = ps[:, s_, :].rearrange("p (t n) -> p t n", t=4)
                              ps2 = ps[:, 2 + s_, :].rearrange("p (t n) -> p t n", t=4)
                              P.op("dve", lambda e: e.tensor_tensor(out=EQ[s_], in0=ps1, in1=V(slotT[:], 0 * 256 + tl, [[1, 4], [0, 128]]), op=ALU.is_equal),
                                   reads=[PB[s_], B_slot], writes=[B_EQ[s_]])
                              P.op("act", lambda e: e.activation(out=E2[s_], in_=ps2, func=AF.Exp), reads=[PB[2 + s_]], writes=[B_E2[s_]])
                              P.op("dve", lambda e: e.tensor_tensor(out=Mk[s_], in0=ps2, in1=V(slotT[:], 1 * 256 + tl, [[1, 4], [0, 128]]), op=ALU.is_ge),
                                   reads=[PB[2 + s_], B_slot, B_E2[s_]], writes=[B_M[s_]])
                              P.op("dve", lambda e: e.tensor_tensor(out=Lm[s_], in0=EQ[s_], in1=V(slotT[:], 2 * 256 + tl, [[1, 4], [0, 128]]), op=ALU.mult),
                                   reads=[B_EQ[s_], B_slot], writes=[B_L[s_]])
                              P.op("dve", lambda e: e.tensor_tensor(out=Rm[s_], in0=Mk[s_], in1=E2[s_], op=ALU.mult), reads=[B_M[s_], B_E2[s_]], writes=[B_R[s_]])

                          def stC(sbi):
                              tl = sbi * 4
                              s_ = sbi % 2

                              def mmg(e):
                                  ins = None
                                  for i in range(4):
                                      ins = e.matmul(ps[:, 4 + s_, i * 128:(i + 1) * 128], lhsT=Rm[s_][:, i, :], rhs=Lm[s_][:, i, :], start=True, stop=True)
                                  return ins
                              P.op("pe", mmg, reads=[B_R[s_], B_L[s_]], writes=[PB[4 + s_]])
                              P.op("act", lambda e: e.activation(out=GT[:, tl:tl + 4, :], in_=ps[:, 4 + s_, :].rearrange("p (t n) -> p t n", t=4), func=AF.Copy),
                                   reads=[PB[4 + s_]], writes=[B_GT])
                          for it in range(NSB + 2):
                              if it < NSB:
                                  stA(it)
                              if 1 <= it <= NSB:
                                  stB(it - 1)
                              if it >= 2:
                                  stC(it - 2)
                          if G == 0:
                              dump(f"GT{l}", GT[:], [B_GT])
                          fence()
                          if G == 0:
                              fill_some(128)
                              P.op("sp", lambda e: e.nop(), extra_deps=list(fill_ops))
                          def pfront(c):
                              sl = c % NSL
                              P.op("sp", lambda e: e.dma_start(out=ub[sl].rearrange("p k n -> p (k n)"), in_=cu_d[c]), writes=[B_ub[sl]], dma_tag=f"cu{sl}")
                              P.op("sp", lambda e: e.dma_start(out=vb[sl], in_=cv_d[c]), writes=[B_vb[sl]], dma_tag=f"cv{sl}")
                              i2 = c % 2
                              bk = 6 + i2

                              def mma(e):
                                  ins = None
                                  for k in range(8):
                                      ins = e.matmul(ps[:, bk, 0:256], lhsT=ub[sl][:, k, :], rhs=h2g[:, k, :], start=(k == 0), stop=(k == 7))
                                  return ins
                              P.op("pe", mma, reads=[B_ub[sl]] + B_h2, writes=[PB[bk]])
                              P.op("act", lambda e: e.activation(out=gl[i2][:], in_=ps[:, bk, 0:256], func=AF.Gelu), reads=[PB[bk]], writes=[B_gl[i2]])
                              P.op("dve", lambda e: e.tensor_tensor(out=actT[i2][:], in0=gl[i2][:], in1=GT[:, :, c], op=ALU.mult),
                                   reads=[B_gl[i2], B_GT], writes=[B_act[i2]])

                          def pback(c):
                              sl = c % NSL
                              i2 = c % 2

                              def mmo2(e):
                                  ins = None
                                  for fk in range(8):
                                      ins = e.matmul(ps[:, fk // 2, (fk % 2) * 256:(fk % 2 + 1) * 256], lhsT=vb[sl][:, fk * 128:(fk + 1) * 128], rhs=actT[i2][:],
                                                     start=(c == 0 and fk % 2 == 0), stop=(c == 127), skip_group_check=True)
                                  return ins
                              P.op("pe", mmo2, reads=[B_vb[sl], B_act[i2]], writes=PB[0:4])
                          for c in range(129):
                              if c < 128:
                                  pfront(c)
                              if c >= 1:
                                  pback(c - 1)
                          for fk in range(8):
                              P.op("dve", lambda e, fk=fk, t0=t0: e.scalar_tensor_tensor(out=xT[:, fk, t0:t0 + 256], in0=ps[:, fk // 2, (fk % 2) * 256:(fk % 2 + 1) * 256],
                                                                                       scalar=modT[:, 40 + fk:41 + fk], in1=xT[:, fk, t0:t0 + 256], op0=ALU.mult, op1=ALU.add),
                                   reads=[PB[fk // 2], B_mod, BX[fk][G]], writes=[BX[fk][G]])
                          P.flush()
                      dump(f"x2_{l}", xT[:], [b for row in BX for b in row])
                      P.barrier()
                      P.flush()
                      P.drop(local_bufs)
                  except _Stop:
                      stopped[0] = True
              if stopped[0]:
                  break
        except _Stop:
            pass

        fin = list(dbg_ops)
        for k in range(8):
            fin.append(P.op("sp", lambda e, k=k: e.dma_start(out=oT_d[k * 128:(k + 1) * 128, :], in_=xT[:, k, :]), reads=BX[k], dma_tag="o"))
        P.flush(final_wait_ops=fin)
    return nc


def _consts():
    ident = np.eye(128, dtype=np.float32)
    slopes = (2.0 ** (-8.0 * np.arange(1, 9) / 8)).astype(np.float64)
    s = np.arange(128)[:, None]
    q = np.arange(128)[None, :]
    biasT = np.zeros((2, 128, 2, 2, 2, 128), np.float32)
    for kh in range(2):
        for g in range(4):
            sl = slopes[kh * 4 + g]
            d0 = q + 128 - s
            d1 = q - s
            biasT[kh, :, g % 2, 0, g // 2, :] = np.where((d0 >= 0) & (d0 < 128), -sl * d0, NEG)
            biasT[kh, :, g % 2, 1, g // 2, :] = np.where((d1 >= 0) & (d1 < 128), -sl * d1, NEG)
    fix = np.ones((128, 4, 16), np.float32)
    for g, w in enumerate((2, 4, 8, 16)):
        t = np.arange(16)
        fix[:, g, :] = (w / np.minimum(t + 1, w))[None, :]
    return ident, biasT.reshape(2, 128, 1024), fix.reshape(128, 64)


def prep_shared(inp):
    f = lambda a: np.ascontiguousarray(a, dtype=np.float32)
    sh = {}
    sh["w_ada"] = f(inp["w_ada"])
    sh["b_adaT"] = f(inp["b_ada"].reshape(NL, 6, 8, 128).transpose(0, 3, 1, 2).reshape(NL, 128, 48))
    gains = np.zeros((NL, 128, 32), np.float32)
    gains[:, :, 0:8] = inp["norm1_g"].reshape(NL, 8, 128).transpose(0, 2, 1)
    gains[:, :, 8:16] = inp["norm2_g"].reshape(NL, 8, 128).transpose(0, 2, 1)
    gains[:, :, 16:24] = inp["mix_norm_g"].reshape(NL, 8, 128).transpose(0, 2, 1)
    gains[:, :, 24:28] = inp["pool_scale"].reshape(NL, 4, 128).transpose(0, 2, 1)
    gains[:, :, 28] = np.tile(inp["q_norm_g"], (1, 2))
    gains[:, :, 29] = np.tile(inp["k_norm_g"], (1, 2))
    sh["gains"] = gains
    sh["sinks"] = f(np.broadcast_to(inp["attn_sinks"][:, None, :], (NL, 128, 8)))
    w_in = inp["w_in"]
    ext = np.concatenate([w_in[:, :, 0:1024], w_in[:, :, 1024:1088], w_in[:, :, 1024:1088], w_in[:, :, 1088:1152], w_in[:, :, 1088:1152],
                          w_in[:, :, 1152:1280]], axis=2)
    chunkify = lambda w, noc: f(w.reshape(NL, 8, 128, noc, 128).transpose(0, 3, 2, 1, 4).reshape(NL, noc, 128, 1024))
    sh["w_in_c"] = chunkify(ext, 11)
    sh["pool_w"] = f(inp["pool_w"])
    sh["w_out_c"] = chunkify(inp["w_out"], 8)
    sh["wq_c"] = chunkify(inp["peer_wq"], 16)
    sh["skT"] = f(inp["peer_subkeys"].transpose(0, 1, 3, 2))
    sh["uT"] = f(inp["peer_u"].reshape(NL, 128, 128, 8, 128).transpose(0, 1, 4, 3, 2).reshape(NL, 128, 128, 1024))
    sh["peer_v"] = f(inp["peer_v"])
    ident, biasT, fix = _consts()
    sh["ident"] = ident
    sh["biasT"] = biasT
    sh["poolfix"] = fix
    return sh


def prep_core(inp, b):
    return {"xT": np.ascontiguousarray(inp["x"][b].T, dtype=np.float32),
            "cT": np.ascontiguousarray(inp["c"][b].reshape(8, 128).T, dtype=np.float32)}


def kernel(**inputs):
    inp = {k: np.asarray(v) for k, v in inputs.items()}
    nb = inp["x"].shape[0]
    sh = prep_shared(inp)
    in_maps = []
    for b in range(nb):
        m = dict(sh)
        m.update(prep_core(inp, b))
        in_maps.append(m)
    nc = build()
    res = run_bass_kernel_spmd(nc, in_maps, core_ids=list(range(nb)))
    out = np.stack([np.asarray(res.results[b]["oT"]).T for b in range(nb)], axis=0)
    return np.ascontiguousarray(out.astype(np.float32))
```
